# Optimizing a Trainium2 kernel written in Bass

```python
import math
import jax, jax.numpy as jnp
from jax import lax
import numpy as np


D_MODEL = 2048
BATCH = 4
SEQ = 4096
DEPTH = 1

HG_HEADS = 8
HG_DK = 128
HG_DV = 128
HG_CHUNK = 64
HG_WIDTH = HG_HEADS * HG_DV
DA_HEADS = 8
DA_DH = 64
DA_DV = 2 * DA_DH
DA_QBLOCK = 128
DA_WIDTH = DA_HEADS * DA_DV
RP_BUCKETS = 32
RP_MAX_EXACT = 16
RP_MAX_DIST = 128
IN_SPLITS = [HG_HEADS * HG_DK, HG_HEADS * HG_DK, HG_WIDTH, HG_WIDTH,
             DA_HEADS * 2 * DA_DH, DA_HEADS * 2 * DA_DH, DA_WIDTH, D_MODEL, D_MODEL]
IN_COLS = sum(IN_SPLITS)
N_EXPERTS = 64
N_GROUPS = 8
TOPK_GROUPS = 4
TOP_K = 8
D_EXPERT = 512
ROUTE_SCALE = 2.5
MOE_BLOCK = 256
EPS = 1e-6

kernel_name = 'hybrid_hgrn2_diffattn_moe_block'

F32 = jnp.float32


def rms_norm(x, g):
    xf = x.astype(F32)
    return xf * lax.rsqrt(jnp.mean(xf * xf, axis=-1, keepdims=True) + EPS) * g.astype(F32)


def modulate(hn, shift, scale):
    return hn * (1.0 + scale[:, None, :].astype(F32)) + shift[:, None, :].astype(F32)


def t5_bucket(n):
    nf = jnp.maximum(n, 1).astype(F32)
    large = RP_MAX_EXACT + (jnp.log(nf / RP_MAX_EXACT) / math.log(RP_MAX_DIST / RP_MAX_EXACT)
                            * (RP_BUCKETS - RP_MAX_EXACT)).astype(jnp.int32)
    large = jnp.minimum(large, RP_BUCKETS - 1)
    return jnp.where(n < RP_MAX_EXACT, n, large)


def hgrn2_branch(q, f_logit, i, g, lb, norm_g):
    bsz, seq, _ = q.shape
    n_chunks = seq // HG_CHUNK
    qs = jax.nn.silu(q.astype(F32)) * HG_DK ** -0.5
    fgate = lb + (1.0 - lb) * jax.nn.sigmoid(f_logit.astype(F32))
    k = 1.0 - fgate
    log_f = jnp.log(fgate)

    def to_chunks(t, d):
        return t.reshape(bsz, n_chunks, HG_CHUNK, HG_HEADS, d).transpose(1, 0, 3, 2, 4)

    qc, kc, gc = to_chunks(qs, HG_DK), to_chunks(k, HG_DK), to_chunks(log_f, HG_DK)
    vc = to_chunks(i.astype(F32), HG_DV)
    causal = jnp.tril(jnp.ones((HG_CHUNK, HG_CHUNK), bool))[:, :, None]

    def step(state, xs):
        qb, kb, vb, gb = xs
        b = jnp.cumsum(gb, axis=2)
        rel = b[:, :, :, None, :] - b[:, :, None, :, :]
        decay = jnp.exp(jnp.where(causal, rel, -jnp.inf))
        scores = jnp.einsum('bhtk,bhtsk,bhsk->bhts', qb, decay, kb)
        o = (jnp.einsum('bhts,bhsv->bhtv', scores, vb)
             + jnp.einsum('bhtk,bhkv->bhtv', qb * jnp.exp(b), state))
        b_last = b[:, :, -1:, :]
        state = (jnp.exp(b_last[:, :, 0, :, None]) * state
                 + jnp.einsum('bhsk,bhsv->bhkv', kb * jnp.exp(b_last - b), vb))
        return state, o

    state0 = jnp.zeros((bsz, HG_HEADS, HG_DK, HG_DV), F32)
    _, o = lax.scan(step, state0, (qc, kc, vc, gc))
    o = o.transpose(1, 0, 3, 2, 4).reshape(bsz, seq, HG_HEADS, HG_DV)
    gate = jax.nn.silu(g.astype(F32)).reshape(bsz, seq, HG_HEADS, HG_DV)
    return (rms_norm(o, norm_g) * gate).reshape(bsz, seq, HG_WIDTH)


def diff_attention_branch(q, k, v, q_g, k_g, lq1, lk1, lq2, lk2, norm_g, rel_table, lambda_init):
    bsz, seq, _ = q.shape

    def heads(t):
        return t.reshape(bsz, seq, DA_HEADS, 2, DA_DH).transpose(0, 2, 3, 1, 4)

    qh = rms_norm(heads(q), q_g) * DA_DH ** -0.5
    kh = rms_norm(heads(k), k_g)
    vh = v.reshape(bsz, seq, DA_HEADS, DA_DV).transpose(0, 2, 1, 3).astype(F32)
    lam = (jnp.exp(jnp.sum(lq1.astype(F32) * lk1.astype(F32)))
           - jnp.exp(jnp.sum(lq2.astype(F32) * lk2.astype(F32))) + lambda_init)
    n_blocks = seq // DA_QBLOCK
    q_blocks = qh.reshape(bsz, DA_HEADS, 2, n_blocks, DA_QBLOCK, DA_DH).transpose(3, 0, 1, 2, 4, 5)
    k_pos = jnp.arange(seq)
    table = rel_table.astype(F32)

    def attend(args):
        qb, blk = args
        q_pos = blk * DA_QBLOCK + jnp.arange(DA_QBLOCK)
        dist = q_pos[:, None] - k_pos[None, :]
        bias = table[t5_bucket(jnp.maximum(dist, 0))].transpose(2, 0, 1)
        logits = jnp.einsum('bhcqd,bhckd->bhcqk', qb, kh) + bias[None, :, None]
        logits = jnp.where(dist >= 0, logits, -jnp.inf)
        p = jax.nn.softmax(logits, axis=-1)
        a = p[:, :, 0] - lam * p[:, :, 1]
        return jnp.einsum('bhqk,bhkv->bhqv', a, vh)

    o = lax.map(attend, (q_blocks, jnp.arange(n_blocks)))
    o = o.transpose(1, 0, 3, 2, 4).reshape(bsz, seq, DA_HEADS, DA_DV)
    o = rms_norm(o, norm_g) * (1.0 - lambda_init)
    return o.reshape(bsz, seq, DA_WIDTH)


def hybrid_mixer(h, w_in, lb, hg_norm_g, q_g, k_g, lq1, lk1, lq2, lk2, da_norm_g, rel_table,
                 w_branch_a, w_branch_b, w_out, lambda_init):
    proj = jnp.einsum('bsd,dn->bsn', h, w_in)
    split_at = np.cumsum(IN_SPLITS)[:-1].tolist()
    hq, hf, hi, hg, dq, dk, dv, ga, gb = jnp.split(proj, split_at, axis=-1)
    ya = jnp.einsum('bsn,nd->bsd', hgrn2_branch(hq, hf, hi, hg, lb, hg_norm_g).astype(h.dtype), w_branch_a)
    yb = jnp.einsum('bsn,nd->bsd', diff_attention_branch(dq, dk, dv, q_g, k_g, lq1, lk1, lq2, lk2,
                                                        da_norm_g, rel_table, lambda_init).astype(h.dtype),
                    w_branch_b)
    merged = (jax.nn.sigmoid(ga.astype(F32)) * ya.astype(F32)
              + jax.nn.sigmoid(gb.astype(F32)) * yb.astype(F32)).astype(h.dtype)
    return jnp.einsum('bsd,de->bse', merged, w_out)


def swiglu(x, wg, wu, wd):
    return (jax.nn.silu(x @ wg) * (x @ wu)) @ wd


def routed_experts(hf, top_e, gate_w, eg, eu, ed):
    n_tok, d = hf.shape
    n_assign = n_tok * TOP_K
    n_blocks = n_assign // MOE_BLOCK + N_EXPERTS + 1
    n_slots = n_blocks * MOE_BLOCK
    flat_e = top_e.reshape(-1)
    flat_tok = jnp.repeat(jnp.arange(n_tok, dtype=jnp.int32), TOP_K)
    flat_w = gate_w.reshape(-1)
    order = jnp.argsort(flat_e, stable=True)
    e_sorted = flat_e[order]
    counts = jnp.bincount(flat_e, length=N_EXPERTS)
    padded = (counts + MOE_BLOCK - 1) // MOE_BLOCK * MOE_BLOCK
    pad_end = jnp.cumsum(padded)
    pad_start = pad_end - padded
    first = jnp.cumsum(counts) - counts
    dest = pad_start[e_sorted] + jnp.arange(n_assign, dtype=jnp.int32) - first[e_sorted]
    slot_tok = jnp.full((n_slots,), n_tok, jnp.int32).at[dest].set(flat_tok[order])
    slot_w = jnp.zeros((n_slots,), F32).at[dest].set(flat_w[order])
    block_e = jnp.minimum(jnp.searchsorted(pad_end, jnp.arange(n_blocks) * MOE_BLOCK, side='right'),
                          N_EXPERTS - 1)
    h_pad = jnp.concatenate([hf, jnp.zeros((1, d), hf.dtype)], axis=0)

    def block(acc, xs):
        e, tok, w = xs
        y = swiglu(h_pad[tok], eg[e], eu[e], ed[e])
        return acc.at[tok].add(y.astype(F32) * w[:, None]), None

    acc, _ = lax.scan(block, jnp.zeros((n_tok + 1, d), F32),
                      (block_e, slot_tok.reshape(n_blocks, MOE_BLOCK), slot_w.reshape(n_blocks, MOE_BLOCK)))
    return acc[:n_tok]


def moe_ffn(h, w_r, b_r, eg, eu, ed, sg, su, sd):
    bsz, seq, d = h.shape
    n_tok = bsz * seq
    hf = h.reshape(n_tok, d)
    scores = jax.nn.sigmoid(jnp.einsum('td,de->te', hf.astype(F32), w_r.astype(F32)))
    sel = scores + b_r.astype(F32)
    group_score = lax.top_k(sel.reshape(n_tok, N_GROUPS, N_EXPERTS // N_GROUPS), 2)[0].sum(-1)
    _, top_groups = lax.top_k(group_score, TOPK_GROUPS)
    group_mask = jax.nn.one_hot(top_groups, N_GROUPS, dtype=F32).sum(1) > 0
    expert_mask = jnp.repeat(group_mask, N_EXPERTS // N_GROUPS, axis=1)
    _, top_e = lax.top_k(jnp.where(expert_mask, sel, -jnp.inf), TOP_K)
    gate_w = jnp.take_along_axis(scores, top_e, axis=1)
    gate_w = gate_w / jnp.sum(gate_w, axis=-1, keepdims=True) * ROUTE_SCALE
    routed = routed_experts(hf, top_e, gate_w, eg, eu, ed)
    out = swiglu(hf, sg, su, sd).astype(F32) + routed
    return out.reshape(bsz, seq, d)


def setup_inputs(seed: int = 0) -> dict:
    key = jax.random.key(seed)
    ks = jax.random.split(key, 32)
    L, D = DEPTH, D_MODEL
    nrm = lambda k, shape, s: jax.random.normal(k, shape, F32) * s
    return {
        'x': nrm(ks[0], (BATCH, SEQ, D), 1.0),
        'c': nrm(ks[1], (BATCH, D), 1.0),
        'ada_w': nrm(ks[2], (L, D, 6 * D), 0.5 * D ** -0.5),
        'ada_b': nrm(ks[3], (L, 6 * D), 0.02),
        'norm1_g': 1.0 + nrm(ks[4], (L, D), 0.02),
        'w_in': nrm(ks[5], (L, D, IN_COLS), D ** -0.5),
        'lb_logits': nrm(ks[6], (L + 1, HG_HEADS * HG_DK), 1.0),
        'hg_norm_g': 1.0 + nrm(ks[7], (L, HG_DV), 0.02),
        'q_norm_g': 1.0 + nrm(ks[8], (L, DA_DH), 0.02),
        'k_norm_g': 1.0 + nrm(ks[9], (L, DA_DH), 0.02),
        'lambda_q1': nrm(ks[10], (L, DA_DH), 0.1),
        'lambda_k1': nrm(ks[11], (L, DA_DH), 0.1),
        'lambda_q2': nrm(ks[12], (L, DA_DH), 0.1),
        'lambda_k2': nrm(ks[13], (L, DA_DH), 0.1),
        'da_norm_g': 1.0 + nrm(ks[14], (L, DA_DV), 0.02),
        'rel_bias': nrm(ks[15], (RP_BUCKETS, DA_HEADS), 0.5),
        'w_branch_a': nrm(ks[16], (L, HG_WIDTH, D), HG_WIDTH ** -0.5),
        'w_branch_b': nrm(ks[17], (L, DA_WIDTH, D), DA_WIDTH ** -0.5),
        'w_out': nrm(ks[18], (L, D, D), D ** -0.5),
        'norm2_g': 1.0 + nrm(ks[19], (L, D), 0.02),
        'router_w': nrm(ks[20], (L, D, N_EXPERTS), D ** -0.5),
        'router_bias': nrm(ks[21], (L, N_EXPERTS), 0.01),
        'w_exp_gate': nrm(ks[22], (L, N_EXPERTS, D, D_EXPERT), D ** -0.5),
        'w_exp_up': nrm(ks[23], (L, N_EXPERTS, D, D_EXPERT), D ** -0.5),
        'w_exp_down': nrm(ks[24], (L, N_EXPERTS, D_EXPERT, D), D_EXPERT ** -0.5),
        'w_sh_gate': nrm(ks[25], (L, D, D_EXPERT), D ** -0.5),
        'w_sh_up': nrm(ks[26], (L, D, D_EXPERT), D ** -0.5),
        'w_sh_down': nrm(ks[27], (L, D_EXPERT, D), D_EXPERT ** -0.5),
    }


def reference(x, c, ada_w, ada_b, norm1_g, w_in, lb_logits, hg_norm_g, q_norm_g, k_norm_g,
              lambda_q1, lambda_k1, lambda_q2, lambda_k2, da_norm_g, rel_bias, w_branch_a,
              w_branch_b, w_out, norm2_g, router_w, router_bias, w_exp_gate, w_exp_up,
              w_exp_down, w_sh_gate, w_sh_up, w_sh_down):
    lower_bounds = jnp.cumsum(jax.nn.softmax(lb_logits.astype(F32), axis=0), axis=0)
    c_act = jax.nn.silu(c)
    for l in range(DEPTH):
        lambda_init = 0.8 - 0.6 * math.exp(-0.3 * l)
        mod = jnp.einsum('bd,dn->bn', c_act, ada_w[l]) + ada_b[l]
        shift1, scale1, gate1, shift2, scale2, gate2 = jnp.split(mod, 6, axis=-1)
        h = modulate(rms_norm(x, norm1_g[l]), shift1, scale1).astype(x.dtype)
        y = hybrid_mixer(h, w_in[l], lower_bounds[l], hg_norm_g[l], q_norm_g[l], k_norm_g[l],
                         lambda_q1[l], lambda_k1[l], lambda_q2[l], lambda_k2[l], da_norm_g[l],
                         rel_bias, w_branch_a[l], w_branch_b[l], w_out[l], lambda_init)
        x = (x.astype(F32) + gate1[:, None, :].astype(F32) * y.astype(F32)).astype(x.dtype)
        h = modulate(rms_norm(x, norm2_g[l]), shift2, scale2).astype(x.dtype)
        y = moe_ffn(h, router_w[l], router_bias[l], w_exp_gate[l], w_exp_up[l], w_exp_down[l],
                    w_sh_gate[l], w_sh_up[l], w_sh_down[l])
        x = (x.astype(F32) + gate2[:, None, :].astype(F32) * y).astype(x.dtype)
    return x
```

```python
import os
import numpy as np
import concourse.bass as bass
import concourse.mybir as mybir
from concourse.bass_utils import run_bass_kernel_spmd

F32 = mybir.dt.float32
BF16 = mybir.dt.bfloat16
I32 = mybir.dt.int32
U32 = mybir.dt.uint32
AF = mybir.ActivationFunctionType
ALU = mybir.AluOpType
AX = mybir.AxisListType

D = 2048
NCH = 16
TOWN = 2048
TWIN = 4096
EPS = 1e-6
NEG = -30000.0


class Tok:
    __slots__ = ("sem", "val")

    def __init__(self, sem, val):
        self.sem = sem
        self.val = val


class Buf:
    __slots__ = ("name", "w", "r", "excl")

    def __init__(self, name="", excl=False):
        self.name = name
        self.w = None
        self.r = {}
        self.excl = excl


class DmaSem:
    __slots__ = ("sem", "total")

    def __init__(self, sem):
        self.sem = sem
        self.total = 0


class Queue:
    def __init__(self, name, eng, sem):
        self.name = name
        self.eng = eng
        self.sem = sem
        self.count = 0
        self.waited = {}
        self.ring = []
        self.ring_i = 0


class Prog:
    def __init__(self, nc, stack):
        self.nc = nc
        self.stack = stack
        self.q = {}
        for name, eng in (("pe", nc.tensor), ("act", nc.scalar), ("dve", nc.vector),
                          ("pool", nc.gpsimd), ("sp", nc.sync)):
            sem = stack.enter_context(nc.semaphore("cs_" + name))
            self.q[name] = Queue(name, eng, sem)
        for name, n in (("sp", 24), ("pool", 24), ("act", 8)):
            for i in range(n):
                sem = stack.enter_context(nc.semaphore(f"ds_{name}{i}"))
                self.q[name].ring.append(DmaSem(sem))
        self.out_toks = []
        self.reg_key = {}
        self.regs = {"pe": stack.enter_context(nc.tensor.register("pe_flag")),
                     "act": stack.enter_context(nc.scalar.register("act_flag")),
                     "dve": stack.enter_context(nc.vector.register("dve_flag"))}
        self.sp_reg = stack.enter_context(nc.sync.register("sp_flag"))

    def _wait(self, q, tok):
        key = id(tok.sem)
        if q.waited.get(key, 0) < tok.val:
            q.eng.wait_ge(tok.sem, tok.val)
            q.waited[key] = tok.val

    def _deps(self, q, reads, writes):
        for b in reads:
            if b.w is not None:
                self._wait(q, b.w)
        for b in writes:
            if b.w is not None:
                self._wait(q, b.w)
            for t in b.r.values():
                self._wait(q, t)

    def _record(self, tok, reads, writes):
        for b in reads:
            k = id(tok.sem)
            o = b.r.get(k)
            if o is None or o.val < tok.val:
                b.r[k] = tok
        for b in writes:
            b.w = tok
            b.r = {}

    def op(self, qname, fn, reads=(), writes=()):
        q = self.q[qname]
        ex = [b for b in reads if b.excl]
        if ex:
            reads = [b for b in reads if not b.excl]
            writes = list(writes) + ex
        self._deps(q, reads, writes)
        ins = fn(q.eng)
        q.count += 1
        ins.then_inc(q.sem, 1)
        if qname == "pe":
            q.waited[id(q.sem)] = q.count
        tok = Tok(q.sem, q.count)
        self._record(tok, reads, writes)
        return tok

    def op_cond(self, flag_ap, fn, reads=(), writes=(), qname="pe"):
        q = self.q[qname]
        eng = q.eng
        ex = [b for b in reads if b.excl]
        if ex:
            reads = [b for b in reads if not b.excl]
            writes = list(writes) + ex
        saved = dict(q.waited)
        reg = self.regs[qname]
        key = (flag_ap.offset, str(flag_ap.ap))
        if self.reg_key.get(qname) != key:
            eng.reg_load(reg, flag_ap)
            self.reg_key[qname] = key
        with eng.If_eq(reg, int(os.environ.get("P6_FLAGVAL", "1"))):
            self._deps(q, reads, writes)
            ins = fn(eng)
            ins.then_inc(q.sem, 1)
        with eng.Else():
            eng.drain()
            eng.sem_inc(q.sem, 1)
        q.count += 1
        q.waited = saved
        if qname == "pe":
            q.waited[id(q.sem)] = q.count
        tok = Tok(q.sem, q.count)
        self._record(tok, reads, writes)
        return tok

    def dma(self, qname, out, in_, reads=(), writes=(), is_out=False, **kw):
        q = self.q[qname]
        self._deps(q, reads, writes)
        ds = q.ring[q.ring_i]
        q.ring_i = (q.ring_i + 1) % len(q.ring)
        if ds.total > 0:
            self._wait(q, Tok(ds.sem, ds.total))
        q.eng.dma_start(out=out, in_=in_, **kw).then_inc(ds.sem, 16)
        ds.total += 16
        tok = Tok(ds.sem, ds.total)
        self._record(tok, reads, writes)
        if is_out:
            self.out_toks.append(tok)
        return tok

    def dma_cond(self, flag_ap, out, in_, reads=(), writes=()):
        q = self.q["sp"]
        eng = q.eng
        ds = q.ring[q.ring_i]
        q.ring_i = (q.ring_i + 1) % len(q.ring)
        if ds.total > 0:
            self._wait(q, Tok(ds.sem, ds.total))
        saved = dict(q.waited)
        key = (flag_ap.offset, str(flag_ap.ap))
        if self.reg_key.get("sp") != key:
            eng.reg_load(self.sp_reg, flag_ap)
            self.reg_key["sp"] = key
        with eng.If_eq(self.sp_reg, int(os.environ.get("P6_DMAFLAG", "1"))):
            self._deps(q, reads, writes)
            eng.dma_start(out=out, in_=in_).then_inc(ds.sem, 16)
        with eng.Else():
            eng.sem_inc(ds.sem, 16)
        q.waited = saved
        ds.total += 16
        tok = Tok(ds.sem, ds.total)
        self._record(tok, reads, writes)
        return tok

    def dma_custom(self, qname, fn, reads=(), writes=()):
        q = self.q[qname]
        self._deps(q, reads, writes)
        ds = q.ring[q.ring_i]
        q.ring_i = (q.ring_i + 1) % len(q.ring)
        if ds.total > 0:
            self._wait(q, Tok(ds.sem, ds.total))
        fn(q.eng).then_inc(ds.sem, 16)
        ds.total += 16
        tok = Tok(ds.sem, ds.total)
        self._record(tok, reads, writes)
        return tok

    def barrier(self):
        toks = []
        for q in self.q.values():
            if q.count > 0:
                toks.append(Tok(q.sem, q.count))
            for ds in q.ring:
                if ds.total > 0:
                    toks.append(Tok(ds.sem, ds.total))
        for q in self.q.values():
            for t in toks:
                if t.sem is not q.sem:
                    self._wait(q, t)

    def finish(self):
        q = self.q["sp"]
        for t in self.out_toks:
            self._wait(q, t)
        self.barrier()


from contextlib import ExitStack

W_HQ, W_HF, W_HI, W_HG, W_DQ, W_DK, W_DV, W_GA, W_GB = 0, 1024, 2048, 3072, 4096, 5120, 6144, 7168, 9216
HG_SCALE = 128 ** -0.5
DA_SCALE = 64 ** -0.5
LAMBDA_INIT = 0.8 - 0.6 * 1.0


class Ctx:
    pass


class StopBuild(Exception):
    pass


_SB_N = [0]


def sb(nc, st, name, shape, dt):
    _SB_N[0] += 1
    return st.enter_context(nc.sbuf_tensor(f"s{_SB_N[0]}_{name}", list(shape), dt))


def build(debug=None):
    nc = bass.Bass("TRN2", target_bir_lowering=False)
    C = Ctx()
    C.nc = nc
    C.debug = debug
    C.in_names = []
    C.dram = {}

    def din(name, shape, dt=F32):
        if name not in C.dram:
            C.dram[name] = nc.dram_tensor(name, list(shape), dt, kind="ExternalInput").ap()
            C.in_names.append(name)
        return C.dram[name]

    def dscr(name, shape, dt):
        if name not in C.dram:
            C.dram[name] = nc.dram_tensor(name, list(shape), dt, kind="Internal").ap()
        return C.dram[name]

    C.din, C.dscr = din, dscr
    C.out = nc.dram_tensor("out", [TOWN, D], F32, kind="ExternalOutput").ap()
    if debug is not None:
        C.dbg = nc.dram_tensor("dbg", list(debug[1]), debug[2], kind="ExternalOutput").ap()
    dscr("Xs", [64 * 1024, D], BF16)
    dscr("Y", [64 * 1024, D], BF16)
    C.mod_d = dscr("mod_d", [1, 6 * D], F32)

    def stop(tag, src=None):
        if debug is not None and debug[0] == tag:
            if src is not None:
                C.P.barrier()
                C.P.dma("sp", C.dbg, src, is_out=True)
            C.P.finish()
            return True
        return False

    with ExitStack() as st:
        P = Prog(nc, st)
        C.P = P
        C.ps = [st.enter_context(nc.psum_tensor(f"ps{i}", [128, 512], F32)) for i in range(6)]
        C.psb = [Buf(f"ps{i}", excl=True) for i in range(6)]
        C.pst = [st.enter_context(nc.psum_tensor(f"pst{i}", [128, 1024], BF16)) for i in range(2)]
        C.pstb = [Buf(f"pst{i}", excl=True) for i in range(2)]
        C.ps_i = 0
        C.pst_i = 0
        load_consts(C, st)
        phase0_mod(C)
        if stop("mod", C.mod_d):
            return nc, C
        C.S32 = sb(nc, st, "S32", [128, 8, 128], F32)
        C.Sbf = sb(nc, st, "Sbf", [128, 8, 128], BF16)
        C.S32b = [Buf() for _ in range(8)]
        C.Sbfb = [Buf() for _ in range(8)]
        P.op("dve", lambda e: e.memset(C.S32[:], 0.0), writes=C.S32b)
        P.op("dve", lambda e: e.memset(C.Sbf[:], 0.0), writes=C.Sbfb)
        half_pass(C, 0)
        half_pass(C, 1)
        if stop("hgrn", C.dscr("hgoT", [128, 8, TOWN], BF16)):
            return nc, C
        if stop("KT", C.dscr("KT", [8, 128, TWIN], BF16)):
            return nc, C
        if stop("V", C.dscr("V", [8, 128, 32, 129], BF16)):
            return nc, C
        if stop("SG", C.dscr("SG", [2, 16, 128, TOWN], BF16)):
            return nc, C
        C.stopped = False
        attention(C)
        if C.stopped:
            return nc, C
        if stop("attn", C.dscr("daoT", [128, 8, TOWN], BF16)):
            return nc, C
        phase4_mix_out(C)
        if C.stopped:
            return nc, C
        if stop("x1", C.dscr("x1", [TOWN, D], F32)):
            return nc, C
        phase5_route(C, st)
        if debug is not None and debug[0] == "route":
            P.barrier()
            P.dma("sp", C.dbg[:, :, 0:8], C.idx_all[:].bitcast(F32).rearrange("p (t k) -> p t k", k=8), is_out=True)
            P.dma("sp", C.dbg[:, :, 8:16], C.w_all[:].rearrange("p (t k) -> p t k", k=8), is_out=True)
            P.finish()
            return nc, C
        phase6_experts(C)
        phase7_combine(C)
        P.finish()
    return nc, C


def next_ps(C):
    i = C.ps_i
    C.ps_i = (i + 1) % len(C.ps)
    return C.ps[i], C.psb[i]


def next_pst(C):
    i = C.pst_i
    C.pst_i = (i + 1) % len(C.pst)
    return C.pst[i], C.pstb[i]


def load_consts(C, st):
    nc, P = C.nc, C.P
    C.ident_f = sb(nc, st, "ident_f", [128, 128], F32)
    C.ident = sb(nc, st, "ident", [128, 128], BF16)
    C.b_ident = Buf()
    P.dma("sp", C.ident_f[:], C.din("ident", [128, 128])[:, :], writes=[C.b_ident])
    P.op("dve", lambda e: e.tensor_copy(out=C.ident[:], in_=C.ident_f[:]), reads=[C.b_ident], writes=[C.b_ident])
    C.cst = sb(nc, st, "cst", [128, 2], F32)
    C.b_cst = Buf()
    P.op("dve", lambda e: e.memset(C.cst[:], EPS), writes=[C.b_cst])
    C.pm = sb(nc, st, "pm", [128, 2], F32)
    C.b_pm = Buf()
    P.dma("sp", C.pm[:], C.din("pm", [128, 2])[:, :], writes=[C.b_pm])


def phase0_mod(C):
    nc, P = C.nc, C.P
    c_col = C.din("c_col", [128, NCH])
    ada_w = C.din("ada_w", [D, 6 * D])
    ada_b = C.din("ada_b", [1, 6 * D])
    with ExitStack() as st:
        ccol = sb(nc, st, "p0_ccol", [128, NCH], F32)
        cact = sb(nc, st, "p0_cact", [128, NCH], BF16)
        wt = [sb(nc, st, f"p0_w{i}", [128, NCH, 512], BF16) for i in range(2)]
        wtb = [Buf() for _ in range(2)]
        bt = sb(nc, st, "p0_b", [1, 6 * D], F32)
        mt = sb(nc, st, "p0_m", [1, 6 * D], F32)
        b_ccol, b_cact, b_bt, b_mt = Buf(), Buf(), Buf(), Buf()
        P.dma("sp", ccol[:], c_col[:, :], writes=[b_ccol])
        P.dma("sp", bt[:], ada_b[:, :], writes=[b_bt])
        P.op("act", lambda e: e.activation(out=cact[:], in_=ccol[:], func=AF.Silu),
             reads=[b_ccol], writes=[b_cact])
        aw = ada_w.rearrange("(c p) n -> p c n", p=128)
        for g in range(24):
            s = g % 2
            P.dma("pool", wt[s][:], aw[:, :, g * 512:(g + 1) * 512], writes=[wtb[s]])
            ps, psb = next_ps(C)

            def mm(e, s=s, ps=ps):
                ins = None
                for j in range(NCH):
                    ins = e.matmul(ps[0:1, :], lhsT=cact[:, j:j + 1], rhs=wt[s][:, j, :],
                                   start=(j == 0), stop=(j == NCH - 1))
                return ins
            P.op("pe", mm, reads=[b_cact, wtb[s]], writes=[psb])
            P.op("dve", lambda e, g=g, ps=ps: e.tensor_tensor(
                out=mt[0:1, g * 512:(g + 1) * 512], in0=ps[0:1, :],
                in1=bt[0:1, g * 512:(g + 1) * 512], op=ALU.add),
                reads=[psb, b_bt], writes=[b_mt])
        P.dma("sp", C.mod_d[:, :], mt[:], reads=[b_mt])
        P.barrier()


def bcast_load(C, dst, src_row):
    return src_row.partition_broadcast(128)


def make_hT(C, st, half, hT, hTb):
    nc, P = C.nc, C.P
    xw = C.din("xw", [TWIN, D])
    norm1_g = C.din("norm1_g", [1, D])
    with ExitStack() as s2:
        G = sb(nc, s2, "n1_G", [128, D], F32)
        S = sb(nc, s2, "n1_S", [128, D], F32)
        t0 = sb(nc, s2, "n1_t0", [128, D], F32)
        bG, bS, bt0 = Buf(), Buf(), Buf()
        P.dma("sp", G[:], C.mod_d[0:1, D:2 * D].partition_broadcast(128), writes=[bG])
        P.dma("sp", t0[:], norm1_g[0:1, :].partition_broadcast(128), writes=[bt0])
        P.dma("sp", S[:], C.mod_d[0:1, 0:D].partition_broadcast(128), writes=[bS])
        P.op("dve", lambda e: e.scalar_tensor_tensor(out=G[:], in0=G[:], scalar=1.0, in1=t0[:],
                                                     op0=ALU.add, op1=ALU.mult),
             reads=[bt0], writes=[bG])
        norm_tiles(C, s2, lambda i: xw[half * TOWN + i * 128: half * TOWN + (i + 1) * 128, :],
                   G, bG, S, bS, hT, hTb, "n1")
        P.barrier()


def norm_tiles(C, s2, src_fn, G, bG, S, bS, hT, hTb, pfx, h32_cb=None, col_fn=None, post_cb=None):
    nc, P = C.nc, C.P
    xt = [sb(nc, s2, f"{pfx}_x{i}", [128, D], F32) for i in range(2)]
    xtb = [Buf() for _ in range(2)]
    junk = sb(nc, s2, f"{pfx}_junk", [128, D], BF16)
    bjunk = Buf()
    tmp = sb(nc, s2, f"{pfx}_tmp", [128, D], F32)
    btmp = Buf()
    hb = [sb(nc, s2, f"{pfx}_hb{i}", [128, D], BF16) for i in range(2)]
    hbb = [Buf() for _ in range(2)]
    ss = sb(nc, s2, f"{pfx}_ss", [128, 4], F32)
    bss = Buf()
    if col_fn is None:
        col_fn = lambda i: i * 128
    for i in range(16):
        s = i % 2
        c0 = col_fn(i)
        P.dma("sp", xt[s][:], src_fn(i), writes=[xtb[s]])
        P.op("act", lambda e, s=s: e.activation(out=junk[:], in_=xt[s][:], func=AF.Square,
                                                accum_out=ss[:, 0:1]),
             reads=[xtb[s]], writes=[bjunk, bss])
        P.op("act", lambda e: e.activation(out=ss[:, 1:2], in_=ss[:, 0:1], func=AF.Ln,
                                           scale=1.0 / D, bias=C.cst[:, 0:1]),
             reads=[bss, C.b_cst], writes=[bss])
        P.op("act", lambda e: e.activation(out=ss[:, 2:3], in_=ss[:, 1:2], func=AF.Exp, scale=-0.5),
             reads=[bss], writes=[bss])
        P.op("dve", lambda e, s=s: e.scalar_tensor_tensor(out=tmp[:], in0=xt[s][:], scalar=ss[:, 2:3],
                                                          in1=G[:], op0=ALU.mult, op1=ALU.mult),
             reads=[xtb[s], bss, bG], writes=[btmp])
        if h32_cb is not None:
            h32_cb(i, tmp, btmp, S, bS, hb[s], hbb[s])
        else:
            P.op("dve", lambda e, s=s: e.tensor_tensor(out=hb[s][:], in0=tmp[:], in1=S[:], op=ALU.add),
                 reads=[btmp, bS], writes=[hbb[s]])
        for hlf in range(2):
            pt, ptb = next_pst(C)

            def tr(e, s=s, hlf=hlf, pt=pt):
                ins = None
                for c in range(8):
                    cc = hlf * 8 + c
                    ins = e.transpose(out=pt[:, c * 128:(c + 1) * 128], in_=hb[s][:, cc * 128:(cc + 1) * 128],
                                      identity=C.ident[:])
                return ins
            P.op("pe", tr, reads=[hbb[s], C.b_ident], writes=[ptb])
            eng = "act" if hlf == 0 else "dve"
            if eng == "act":
                P.op("act", lambda e, hlf=hlf, pt=pt, c0=c0: e.copy(
                    out=hT[:, hlf * 8:(hlf + 1) * 8, c0:c0 + 128],
                    in_=pt[:].rearrange("p (c t) -> p c t", t=128)),
                    reads=[ptb], writes=[hTb[i]])
            else:
                P.op("dve", lambda e, hlf=hlf, pt=pt, c0=c0: e.tensor_copy(
                    out=hT[:, hlf * 8:(hlf + 1) * 8, c0:c0 + 128],
                    in_=pt[:].rearrange("p (c t) -> p c t", t=128)),
                    reads=[ptb], writes=[hTb[i]])
        if post_cb is not None:
            post_cb(i, hb[s], hbb[s], c0)


def fm_proj(C, w, wb, c0, hT, hTb, tg, ncol=128):
    P = C.P
    ps, psb = next_ps(C)

    def mm(e):
        ins = None
        for j in range(NCH):
            ins = e.matmul(ps[0:ncol, :], lhsT=w[:, j, c0:c0 + ncol], rhs=hT[:, j, tg * 512:(tg + 1) * 512],
                           start=(j == 0), stop=(j == NCH - 1))
        return ins
    P.op("pe", mm, reads=[wb] + hTb[tg * 4:(tg + 1) * 4], writes=[psb])
    return ps, psb


def load_wcols(C, slot, slotb, col0, ncol):
    w_in = C.din("w_in", [D, 11264])
    wv = w_in.rearrange("(c p) n -> p c n", p=128)
    C.P.dma("pool", slot[:, :, 0:ncol], wv[:, :, col0:col0 + ncol], writes=[slotb])


def half_pass(C, half):
    nc, P = C.nc, C.P
    with ExitStack() as st:
        hT = sb(nc, st, "hT", [128, NCH, TOWN], BF16)
        hTb = [Buf() for _ in range(16)]
        make_hT(C, st, half, hT, hTb)
        if C.debug is not None and C.debug[0] == "hT" and half == 1:
            P.barrier()
            P.dma("sp", C.dbg, hT[:], is_out=True)
            return
        with ExitStack() as s2:
            hgrn_half(C, s2, half, hT, hTb)
            P.barrier()
        with ExitStack() as s2:
            attn_proj_half(C, s2, half, hT, hTb)
            P.barrier()
        if half == 1:
            with ExitStack() as s2:
                gates_proj(C, s2, hT, hTb)
                P.barrier()
        P.barrier()


def hgrn_half(C, st, half, hT, hTb):
    nc, P = C.nc, C.P
    own = half == 1
    T = TOWN
    NC64 = T // 64
    lbl = sb(nc, st, "hg_lbl", [128, 2, 8], F32)
    lb = sb(nc, st, "hg_lb", [128, 8], F32)
    oml = sb(nc, st, "hg_oml", [128, 8], F32)
    cmask = sb(nc, st, "hg_cmask", [128, T], F32)
    mask64 = sb(nc, st, "hg_mask64", [64, 64], F32)
    hgg = sb(nc, st, "hg_g", [64, 128], F32)
    b_c = Buf()
    P.dma("sp", lbl[:], C.din("lb_l", [128, 2, 8])[:, :, :], writes=[b_c])
    P.dma("sp", cmask[:], C.din("cmask", [128, T])[:, :], writes=[b_c])
    P.dma("sp", mask64[:], C.din("mask64", [64, 64])[:, :], writes=[b_c])
    P.dma("sp", hgg[:], C.din("hg_norm_g", [1, 128])[0:1, :].partition_broadcast(64), writes=[b_c])
    P.op("dve", lambda e: e.tensor_tensor(out=oml[:], in0=lbl[:, 0, :], in1=lbl[:, 1, :], op=ALU.subtract),
         reads=[b_c], writes=[b_c])
    P.op("act", lambda e: e.activation(out=lb[:], in_=oml[:], func=AF.Sigmoid), reads=[b_c], writes=[b_c])
    P.op("dve", lambda e: e.tensor_scalar(out=oml[:], in0=lb[:], scalar1=-1.0, scalar2=1.0,
                                          op0=ALU.mult, op1=ALU.add), reads=[b_c], writes=[b_c])
    def f32arr(n):
        return sb(nc, st, "hg_" + n, [128, T], F32), Buf()
    A, bA = f32arr("A")
    G, bG = f32arr("G")
    K, bK = f32arr("K")
    B, bB = f32arr("B")
    Dd, bD = A, bA
    E, bE = f32arr("E")
    if own:
        Q, bQ = f32arr("Q")
    sg = [sb(nc, st, f"hg_sg{i}", [128, 512], F32) for i in range(2)]
    sgb = [Buf() for _ in range(2)]
    fmT = [sg[i][:].bitcast(BF16)[:, 0:512] for i in range(2)]
    fmTb = sgb
    khatT = sb(nc, st, "hg_khatT", [128, T], BF16); b_khatT = Buf()
    khat = sb(nc, st, "hg_khat", [64, NC64, 128], BF16); b_khat = Buf()
    vh = sb(nc, st, "hg_vh", [64, NC64, 128], BF16); b_vh = Buf()
    dec = sb(nc, st, "hg_dec", [128, NC64], F32); b_dec = Buf()
    if own:
        qhat = sb(nc, st, "hg_qhat", [128, T], BF16); b_qhat = Buf()
        qtil = sb(nc, st, "hg_qtil", [128, T], BF16); b_qtil = Buf()
        ktil = sb(nc, st, "hg_ktil", [128, T], BF16); b_ktil = Buf()
        gate = sb(nc, st, "hg_gate", [64, NC64, 128], BF16); b_gate = Buf()
        STs = [sb(nc, st, f"hg_ST{i}", [64, 64], BF16) for i in range(2)]
        STb = [Buf() for _ in range(2)]
        hgoT, b_hgoT = khatT, b_khatT
        og = [sb(nc, st, f"hg_og{i}", [64, 128], F32) for i in range(2)]
        ogb = [Buf() for _ in range(2)]
        hgo = [sb(nc, st, f"hg_hgo{i}", [64, 128], BF16) for i in range(2)]
        hgob = [Buf() for _ in range(2)]
        oss = sb(nc, st, "hg_oss", [64, NC64], F32); b_oss = Buf()
        o_raw_p = [A[0:64, :].rearrange("p (n c) -> p n c", c=128), K[0:64, :].rearrange("p (n c) -> p n c", c=128)]
        b_oraw_p = [bA, bK]
        o_sq = E[0:64, :].rearrange("p (n c) -> p n c", c=128); b_osq = bE
        hgo_all = G[0:64, :].bitcast(BF16).rearrange("p (n c) -> p n c", c=128); b_hgoall = bG
        ojunk = sb(nc, st, "hg_ojunk", [64, 128], BF16); b_ojunk = Buf()
        hgoT_d = C.dscr("hgoT", [128, 8, TOWN], BF16)
    nw = 4 if own else 2
    wsl = [[sb(nc, st, f"hg_w{k}_{i}", [128, NCH, 128], BF16) for i in range(2)] for k in range(nw)]
    wslb = [[Buf() for i in range(2)] for k in range(nw)]
    fam_cols = [W_HF, W_HI, W_HQ, W_HG]

    def b3(ap):
        return ap.rearrange("p (n c) -> p n c", c=64)

    for h in range(8):
        s = h % 2
        for k in range(nw):
            load_wcols(C, wsl[k][s], wslb[k][s], fam_cols[k] + h * 128, 128)
        w_hf, w_hi = wsl[0][s], wsl[1][s]
        for tg in range(4):
            sl = slice(tg * 512, (tg + 1) * 512)
            ps, psb = fm_proj(C, w_hf, wslb[0][s], 0, hT, hTb, tg)
            P.op("act", lambda e, ps=ps, tg=tg: e.activation(out=sg[tg % 2][:], in_=ps[:], func=AF.Sigmoid),
                 reads=[psb], writes=[sgb[tg % 2]])
            P.op("dve", lambda e, tg=tg, sl=sl, h=h: e.tensor_scalar(
                out=A[:, sl], in0=sg[tg % 2][:], scalar1=oml[:, h:h + 1], scalar2=lb[:, h:h + 1],
                op0=ALU.mult, op1=ALU.add), reads=[sgb[tg % 2], b_c], writes=[bA])
            P.op("act", lambda e, sl=sl: e.activation(out=G[:, sl], in_=A[:, sl], func=AF.Ln),
                 reads=[bA], writes=[bG])
            P.op("dve", lambda e, sl=sl: e.tensor_scalar(out=K[:, sl], in0=A[:, sl], scalar1=-1.0, scalar2=1.0,
                                                         op0=ALU.mult, op1=ALU.add), reads=[bA], writes=[bK])
            if own:
                ps, psb = fm_proj(C, wsl[2][s], wslb[2][s], 0, hT, hTb, tg)
                P.op("act", lambda e, ps=ps, sl=sl: e.activation(out=Q[:, sl], in_=ps[:], func=AF.Silu),
                     reads=[psb], writes=[bQ])
        P.op("dve", lambda e: e.tensor_tensor_scan(out=B[:], data0=cmask[:], data1=G[:], initial=0.0,
                                                   op0=ALU.mult, op1=ALU.add), reads=[b_c, bG], writes=[bB])
        P.op("dve", lambda e: e.tensor_tensor(out=b3(Dd[:]), in0=b3(B[:])[:, :, 63:64].to_broadcast([128, NC64, 64]),
                                              in1=b3(B[:]), op=ALU.subtract), reads=[bB], writes=[bD])
        P.op("act", lambda e: e.activation(out=E[:], in_=Dd[:], func=AF.Exp), reads=[bD], writes=[bE])
        pmc = 1 if own else 0
        P.op("dve", lambda e, pmc=pmc: e.scalar_tensor_tensor(out=khatT[:], in0=K[:], scalar=C.pm[:, pmc:pmc + 1],
                                                              in1=E[:], op0=ALU.mult, op1=ALU.mult),
             reads=[bK, bE, C.b_pm], writes=[b_khatT])
        P.op("act", lambda e: e.activation(out=dec[:], in_=b3(B[:])[:, :, 63], func=AF.Exp), reads=[bB], writes=[b_dec])
        for c8 in range(NC64 // 8):
            pt, ptb = next_pst(C)

            def tr(e, c8=c8, pt=pt):
                ins = None
                for c in range(8):
                    cc = c8 * 8 + c
                    ins = e.transpose(out=pt[0:64, c * 128:(c + 1) * 128], in_=khatT[:, cc * 64:(cc + 1) * 64],
                                      identity=C.ident[:])
                return ins
            P.op("pe", tr, reads=[b_khatT, C.b_ident], writes=[ptb])
            P.op("act", lambda e, c8=c8, pt=pt: e.copy(out=khat[:, c8 * 8:(c8 + 1) * 8, :],
                                                       in_=pt[0:64, :].rearrange("p (c k) -> p c k", k=128)),
                 reads=[ptb], writes=[b_khat])
        if own:
            P.op("act", lambda e: e.activation(out=E[:], in_=B[:], func=AF.Exp), reads=[bB], writes=[bE])
            P.op("dve", lambda e: e.scalar_tensor_tensor(out=qhat[:], in0=Q[:], scalar=HG_SCALE, in1=E[:],
                                                         op0=ALU.mult, op1=ALU.mult), reads=[bQ, bE], writes=[b_qhat])
            P.op("dve", lambda e: e.tensor_tensor(out=b3(Dd[:]), in0=b3(B[:]),
                                                  in1=b3(B[:])[:, :, 31:32].to_broadcast([128, NC64, 64]),
                                                  op=ALU.subtract), reads=[bB], writes=[bD])
            P.op("act", lambda e: e.activation(out=E[:], in_=Dd[:], func=AF.Exp), reads=[bD], writes=[bE])
            P.op("dve", lambda e: e.scalar_tensor_tensor(out=qtil[:], in0=Q[:], scalar=HG_SCALE, in1=E[:],
                                                         op0=ALU.mult, op1=ALU.mult), reads=[bQ, bE], writes=[b_qtil])
            P.op("act", lambda e: e.activation(out=E[:], in_=Dd[:], func=AF.Exp, scale=-1.0), reads=[bD], writes=[bE])
            P.op("dve", lambda e: e.tensor_tensor(out=ktil[:], in0=K[:], in1=E[:], op=ALU.mult),
                 reads=[bK, bE], writes=[b_ktil])
        fams = [(w_hi, wslb[1][s], vh, b_vh, AF.Copy)]
        if own:
            fams.append((wsl[3][s], wslb[3][s], gate, b_gate, AF.Silu))
        for (w, wb, dst, dstb, fn) in fams:
            for tg in range(4):
                ps, psb = fm_proj(C, w, wb, 0, hT, hTb, tg)
                fi = tg % 2
                P.op("act", lambda e, ps=ps, fi=fi, fn=fn: e.activation(out=fmT[fi], in_=ps[:], func=fn),
                     reads=[psb], writes=[fmTb[fi]])
                pt, ptb = next_pst(C)

                def trv(e, pt=pt, fi=fi):
                    ins = None
                    for c in range(8):
                        ins = e.transpose(out=pt[0:64, c * 128:(c + 1) * 128], in_=fmT[fi][:, c * 64:(c + 1) * 64],
                                          identity=C.ident[:])
                    return ins
                P.op("pe", trv, reads=[fmTb[fi], C.b_ident], writes=[ptb])
                P.op("dve", lambda e, pt=pt, tg=tg, dst=dst: e.tensor_copy(
                    out=dst[:, tg * 8:(tg + 1) * 8, :], in_=pt[0:64, :].rearrange("p (c k) -> p c k", k=128)),
                    reads=[ptb], writes=[dstb])
        for c in range(NC64):
            cs = slice(c * 64, (c + 1) * 64)
            if own:
                ps_st, psb_st = next_ps(C)
                P.op("pe", lambda e, ps_st=ps_st, cs=cs: e.matmul(ps_st[0:64, 0:64], lhsT=ktil[:, cs], rhs=qtil[:, cs],
                                                                start=True, stop=True),
                     reads=[b_ktil, b_qtil], writes=[psb_st])
                si = c % 2
                P.op("dve", lambda e, ps_st=ps_st, si=si: e.tensor_tensor(out=STs[si][:], in0=ps_st[0:64, 0:64],
                                                                         in1=mask64[:], op=ALU.mult),
                     reads=[psb_st, b_c], writes=[STb[si]])
                ps_o, psb_o = next_ps(C)

                def mmo(e, ps_o=ps_o, si=si, c=c, cs=cs, h=h):
                    e.matmul(ps_o[0:64, 0:128], lhsT=STs[si][:], rhs=vh[:, c, :], start=True, stop=False)
                    return e.matmul(ps_o[0:64, 0:128], lhsT=qhat[:, cs], rhs=C.Sbf[:, h, :], start=False, stop=True)
                P.op("pe", mmo, reads=[STb[si], b_vh, b_qhat, C.Sbfb[h]], writes=[psb_o])
            ps_kv, psb_kv = next_ps(C)
            P.op("pe", lambda e, ps_kv=ps_kv, c=c: e.matmul(ps_kv[:, 0:128], lhsT=khat[:, c, :], rhs=vh[:, c, :],
                                                           start=True, stop=True),
                 reads=[b_khat, b_vh], writes=[psb_kv])
            P.op("dve", lambda e, ps_kv=ps_kv, c=c, h=h: e.scalar_tensor_tensor(
                out=C.S32[:, h, :], in0=C.S32[:, h, :], scalar=dec[:, c:c + 1], in1=ps_kv[:, 0:128],
                op0=ALU.mult, op1=ALU.add), reads=[psb_kv, b_dec], writes=[C.S32b[h]])
            P.op("dve", lambda e, h=h: e.tensor_copy(out=C.Sbf[:, h, :], in_=C.S32[:, h, :]),
                 reads=[C.S32b[h]], writes=[C.Sbfb[h]])
            if own:
                P.op("act", lambda e, ps_o=ps_o, c=c: e.copy(out=o_raw_p[c // 16][:, c % 16, :], in_=ps_o[0:64, 0:128]),
                     reads=[psb_o], writes=[b_oraw_p[c // 16]])
        if own:
            HN = NC64 // 2
            for hf_ in range(2):
                cs_ = slice(hf_ * HN, (hf_ + 1) * HN)
                P.op("dve", lambda e, hf_=hf_: e.tensor_tensor(out=o_sq, in0=o_raw_p[hf_], in1=o_raw_p[hf_], op=ALU.mult),
                     reads=[b_oraw_p[hf_]], writes=[b_osq])
                P.op("dve", lambda e, cs_=cs_: e.tensor_reduce(out=oss[:, cs_], in_=o_sq, axis=AX.X, op=ALU.add), reads=[b_osq], writes=[b_oss])
            P.op("act", lambda e: e.activation(out=oss[:, 0:NC64], in_=oss[:, 0:NC64], func=AF.Ln, scale=1.0 / 128,
                                               bias=C.cst[0:64, 0:1]), reads=[C.b_cst], writes=[b_oss])
            P.op("act", lambda e: e.activation(out=oss[:, 0:NC64], in_=oss[:, 0:NC64], func=AF.Exp, scale=-0.5), reads=[], writes=[b_oss])
            for hf_ in range(2):
                cs_ = slice(hf_ * HN, (hf_ + 1) * HN)
                P.op("dve", lambda e, cs_=cs_, hf_=hf_: e.tensor_tensor(out=o_sq, in0=o_raw_p[hf_],
                                                                        in1=oss[:, cs_].unsqueeze(2).to_broadcast([64, HN, 128]), op=ALU.mult),
                     reads=[b_oraw_p[hf_], b_oss], writes=[b_osq])
                P.op("dve", lambda e: e.tensor_tensor(out=o_sq, in0=o_sq, in1=hgg[:].unsqueeze(1).to_broadcast([64, HN, 128]), op=ALU.mult),
                     reads=[b_c], writes=[b_osq])
                P.op("dve", lambda e, cs_=cs_: e.tensor_tensor(out=hgo_all[:, cs_, :], in0=o_sq, in1=gate[:, cs_, :], op=ALU.mult),
                     reads=[b_osq, b_gate], writes=[b_hgoall])
            for c8 in range(NC64 // 8):
                pt_o, ptb_o = next_pst(C)

                def tro(e, c8=c8, pt_o=pt_o):
                    ins = None
                    for c in range(8):
                        ins = e.transpose(out=pt_o[:, c * 64:(c + 1) * 64], in_=hgo_all[:, c8 * 8 + c, :], identity=C.ident[0:64, 0:64])
                    return ins
                P.op("pe", tro, reads=[b_hgoall, C.b_ident], writes=[ptb_o])
                P.op("act", lambda e, pt_o=pt_o, c8=c8: e.copy(out=hgoT[:, c8 * 512:(c8 + 1) * 512], in_=pt_o[:, 0:512]),
                     reads=[ptb_o], writes=[b_hgoT])
        if own:
            P.dma("sp", hgoT_d[:, h, :], hgoT[:], reads=[b_hgoT], writes=[])


def host_array(name, inputs, core):
    b, half = core // 2, core % 2
    f = np.float32
    if name == "xw":
        x = inputs["x"]
        xw = np.zeros((TWIN, D), f)
        if half == 1:
            xw[:] = x[b]
        else:
            xw[TOWN:] = x[b, :TOWN]
        return xw
    if name == "c_col":
        return np.ascontiguousarray(np.asarray(inputs["c"][b], f).reshape(NCH, 128).T)
    if name == "ada_b":
        return np.ascontiguousarray(inputs["ada_b"][0][None, :])
    if name in ("ada_w", "w_in", "w_branch_a", "w_branch_b", "w_out", "w_sh_gate", "w_sh_up", "w_sh_down",
                "w_exp_gate", "w_exp_up", "w_exp_down", "router_w"):
        return np.ascontiguousarray(inputs[name][0])
    if name in ("norm1_g", "norm2_g", "hg_norm_g", "da_norm_g", "router_bias"):
        return np.ascontiguousarray(np.asarray(inputs[name][0], f)[None, :])
    if name == "ident":
        return np.eye(128, dtype=f)
    if name == "pm":
        pm = np.ones((128, 2), f)
        pm[:, 0] = float(half)
        return pm
    if name == "lb_l":
        l = np.asarray(inputs["lb_logits"], f)
        return np.ascontiguousarray(l.reshape(2, 8, 128).transpose(2, 0, 1))
    if name == "cmask":
        m = np.ones((128, TOWN), f)
        m[:, ::64] = 0.0
        return m
    if name == "blockones":
        m = np.zeros((128, 128), f)
        m[:64, :64] = 1.0
        m[64:, 64:] = 1.0
        return m
    if name == "qk_g":
        g = np.zeros((128, 2), f)
        g[:, 0] = np.tile(np.asarray(inputs["k_norm_g"][0], f), 2)
        g[:, 1] = np.tile(np.asarray(inputs["q_norm_g"][0], f), 2)
        return g
    if name == "abias":
        tab = np.asarray(inputs["rel_bias"], f)
        r = np.arange(128)[:, None]
        u = np.arange(1024)[None, :]
        dist = u - r - 384
        n = np.maximum(dist, 1).astype(np.float32)
        large = 16 + (np.log(n / 16) / np.log(128 / 16) * 16).astype(np.int32)
        large = np.minimum(large, 31)
        bucket = np.where(dist < 16, np.maximum(dist, 0), large)
        out = np.empty((8, 128, 1024), f)
        for hh in range(8):
            out[hh] = np.where(dist >= 0, tab[bucket, hh], f(NEG))
        return out
    if name == "farb":
        tab = np.asarray(inputs["rel_bias"], f)
        o = np.empty((128, 17), f)
        for hh in range(8):
            o[:, 2 * hh] = tab[31, hh] if half == 1 else f(NEG)
            o[:, 2 * hh + 1] = tab[31, hh]
        o[:, 16] = 0.0 if half == 1 else f(NEG)
        return o
    if name == "lam4":
        return np.stack([np.asarray(inputs[k][0], f) for k in ("lambda_q1", "lambda_k1", "lambda_q2", "lambda_k2")])
    if name == "thr8":
        return np.tile(np.arange(8, dtype=f) * 128.0, 64)[None, :]
    if name == "ecap":
        return (np.arange(64, dtype=f) * CAP)[None, :]
    if name == "ustrict":
        t = np.arange(128)
        return (t[:, None] < t[None, :]).astype(f)
    if name == "mask64":
        s = np.arange(64)
        return (s[:, None] <= s[None, :]).astype(f)
    raise KeyError(name)


def make_in_maps(C, inputs, cores):
    inputs = {k: np.asarray(v) for k, v in inputs.items()}
    return [{n: host_array(n, inputs, core) for n in C.in_names} for core in cores]


def kernel(**inputs):
    nc, C = build()
    maps = make_in_maps(C, inputs, list(range(8)))
    res = run_bass_kernel_spmd(nc, maps, core_ids=list(range(8)))
    out = np.zeros((4, 4096, D), np.float32)
    for core in range(8):
        b, half = core // 2, core % 2
        out[b, half * TOWN:(half + 1) * TOWN] = np.asarray(res.results[core]["out"])
    return out


def attn_proj_half(C, st, half, hT, hTb):
    nc, P = C.nc, C.P
    own = half == 1
    KT_d = C.dscr("KT", [8, 128, TWIN], BF16)
    QT_d = C.dscr("QT", [8, 128, TOWN], BF16)
    V_d = C.dscr("V", [8, 128, 32, 129], BF16)
    bones = sb(nc, st, "ap_bones", [128, 128], BF16)
    bones_f = sb(nc, st, "ap_bones_f", [128, 128], F32)
    gcol = sb(nc, st, "ap_gcol", [128, 2], F32)
    b_c = Buf()
    P.dma("sp", bones_f[:], C.din("blockones", [128, 128])[:, :], writes=[b_c])
    P.op("dve", lambda e: e.tensor_copy(out=bones[:], in_=bones_f[:]), reads=[b_c], writes=[b_c])
    P.dma("sp", gcol[:], C.din("qk_g", [128, 2])[:, :], writes=[b_c])
    P.op("dve", lambda e: e.tensor_scalar(out=gcol[:, 1:2], in0=gcol[:, 1:2], scalar1=DA_SCALE, scalar2=None,
                                          op0=ALU.mult), reads=[b_c], writes=[b_c])
    nw = 3 if own else 2
    wsl = [[sb(nc, st, f"ap_w{k}_{i}", [128, NCH, 128], BF16) for i in range(2)] for k in range(nw)]
    wslb = [[Buf() for i in range(2)] for k in range(nw)]
    fam_cols = [W_DK, W_DV, W_DQ]
    sq = [sb(nc, st, f"ap_sq{i}", [128, 512], BF16) for i in range(2)]
    sqb = [Buf() for _ in range(2)]
    raw = [sb(nc, st, f"ap_raw{i}", [128, 512], F32) for i in range(2)]
    rawb = [Buf() for _ in range(2)]
    rr = [sb(nc, st, f"ap_rr{i}", [128, 512], F32) for i in range(2)]
    rrb = [Buf() for _ in range(2)]
    xn = [sb(nc, st, f"ap_xn{i}", [128, TOWN], BF16) for i in range(2)]
    xnb = [Buf() for _ in range(2)]
    fmT = [sb(nc, st, f"ap_fmT{i}", [128, 512], BF16) for i in range(2)]
    fmTb = [Buf() for _ in range(2)]
    Vs = [sb(nc, st, f"ap_V{i}", [128, 16, 129], BF16) for i in range(2)]
    Vsb = [Buf() for _ in range(2)]
    for i in range(2):
        P.op("dve", lambda e, i=i: e.memset(Vs[i][:, :, 128:129], 1.0), writes=[Vsb[i]])
    it = 0
    for h in range(8):
        s = h % 2
        for k in range(nw):
            load_wcols(C, wsl[k][s], wslb[k][s], fam_cols[k] + h * 128, 128)
        for (k, gc, dst) in ([(0, 0, KT_d[h, :, half * TOWN:(half + 1) * TOWN])] + ([(2, 1, QT_d[h, :, :])] if own else [])):
            xi = it % 2
            it += 1
            for tg in range(4):
                sl = slice(tg * 512, (tg + 1) * 512)
                ps, psb = fm_proj(C, wsl[k][s], wslb[k][s], 0, hT, hTb, tg)
                ti = tg % 2
                P.op("act", lambda e, ps=ps, ti=ti: e.activation(out=sq[ti][:], in_=ps[:], func=AF.Square),
                     reads=[psb], writes=[sqb[ti]])
                P.op("dve", lambda e, ps=ps, ti=ti: e.tensor_copy(out=raw[ti][:], in_=ps[:]),
                     reads=[psb], writes=[rawb[ti]])
                ps2, psb2 = next_ps(C)
                P.op("pe", lambda e, ps2=ps2, ti=ti: e.matmul(ps2[:], lhsT=bones[:], rhs=sq[ti][:], start=True, stop=True),
                     reads=[sqb[ti], b_c], writes=[psb2])
                P.op("act", lambda e, ps2=ps2, ti=ti: e.activation(out=rr[ti][:], in_=ps2[:], func=AF.Ln, scale=1.0 / 64,
                                                                  bias=C.cst[:, 0:1]), reads=[psb2, C.b_cst], writes=[rrb[ti]])
                P.op("act", lambda e, ti=ti: e.activation(out=rr[ti][:], in_=rr[ti][:], func=AF.Exp, scale=-0.5),
                     reads=[], writes=[rrb[ti]])
                P.op("dve", lambda e, ti=ti, sl=sl, xi=xi, gc=gc: e.scalar_tensor_tensor(
                    out=xn[xi][:, sl], in0=raw[ti][:], scalar=gcol[:, gc:gc + 1], in1=rr[ti][:],
                    op0=ALU.mult, op1=ALU.mult), reads=[rawb[ti], rrb[ti], b_c], writes=[xnb[xi]])
            P.dma("sp", dst, xn[xi][:], reads=[xnb[xi]])
        vi = h % 2
        for tg in range(4):
            ps, psb = fm_proj(C, wsl[1][s], wslb[1][s], 0, hT, hTb, tg)
            fi = tg % 2
            P.op("act", lambda e, ps=ps, fi=fi: e.copy(out=fmT[fi][:], in_=ps[:]), reads=[psb], writes=[fmTb[fi]])
            pt, ptb = next_pst(C)

            def trv(e, pt=pt, fi=fi):
                ins = None
                for t in range(4):
                    ins = e.transpose(out=pt[:, t * 128:(t + 1) * 128], in_=fmT[fi][:, t * 128:(t + 1) * 128], identity=C.ident[:])
                return ins
            P.op("pe", trv, reads=[fmTb[fi], C.b_ident], writes=[ptb])
            P.op("dve", lambda e, pt=pt, tg=tg, vi=vi: e.tensor_copy(
                out=Vs[vi][:, tg * 4:(tg + 1) * 4, 0:128], in_=pt[:, 0:512].rearrange("p (t k) -> p t k", k=128)),
                reads=[ptb], writes=[Vsb[vi]])
        P.dma("sp", V_d[h, :, half * 16:(half + 1) * 16, :], Vs[vi][:], reads=[Vsb[vi]])


def gates_proj(C, st, hT, hTb):
    nc, P = C.nc, C.P
    SG_d = C.dscr("SG", [2, 16, 128, TOWN], BF16)
    wsl = [sb(nc, st, f"gp_w{i}", [128, NCH, 512], BF16) for i in range(2)]
    wslb = [Buf() for _ in range(2)]
    sg = [sb(nc, st, f"gp_sg{i}", [128, TOWN], BF16) for i in range(2)]
    sgb = [Buf() for _ in range(2)]
    n = 0
    for fam in range(2):
        for cg in range(4):
            s = (fam * 4 + cg) % 2
            load_wcols(C, wsl[s], wslb[s], (W_GA if fam == 0 else W_GB) + cg * 512, 512)
            for cc in range(4):
                si = n % 2
                n += 1
                for tg in range(4):
                    ps, psb = fm_proj(C, wsl[s], wslb[s], cc * 128, hT, hTb, tg)
                    P.op("act", lambda e, ps=ps, si=si, tg=tg: e.activation(
                        out=sg[si][:, tg * 512:(tg + 1) * 512], in_=ps[:], func=AF.Sigmoid),
                        reads=[psb], writes=[sgb[si]])
                P.dma("sp", SG_d[fam, cg * 4 + cc, :, :], sg[si][:], reads=[sgb[si]])


def attention(C):
    nc, P = C.nc, C.P
    KT_d = C.dscr("KT", [8, 128, TWIN], BF16)
    QT_d = C.dscr("QT", [8, 128, TOWN], BF16)
    V_d = C.dscr("V", [8, 128, 32, 129], BF16)
    daoT_d = C.dscr("daoT", [128, 8, TOWN], BF16)
    abias = C.din("abias", [8, 128, 1024])
    farb = C.din("farb", [128, 17])
    lam4 = C.din("lam4", [4, 64])
    with ExitStack() as st:
        KT = [sb(nc, st, f"at_KT{i}", [128, TWIN], BF16) for i in range(2)]
        QT = [sb(nc, st, f"at_QT{i}", [128, TOWN], BF16) for i in range(2)]
        V = [sb(nc, st, f"at_V{i}", [128, 32, 129], BF16) for i in range(2)]
        Wb = [sb(nc, st, f"at_Wb{i}", [128, 1024], F32) for i in range(2)]
        hb = [Buf() for _ in range(2)]
        fb = sb(nc, st, "at_farb", [128, 17], F32)
        l4 = sb(nc, st, "at_l4", [128, 4, 64], F32)
        lt = sb(nc, st, "at_lt", [128, 2, 64], F32)
        lam = sb(nc, st, "at_lam", [128, 4], F32)
        dag = sb(nc, st, "at_dag", [128, 128], F32)
        b_c = Buf()
        P.dma("sp", fb[:], farb[:, :], writes=[b_c])
        for i in range(4):
            P.dma("sp", l4[:, i, :], lam4[i:i + 1, :].partition_broadcast(128), writes=[b_c])
        P.dma("sp", dag[:], C.din("da_norm_g", [1, 128])[0:1, :].partition_broadcast(128), writes=[b_c])
        P.op("dve", lambda e: e.tensor_scalar(out=dag[:], in0=dag[:], scalar1=1.0 - LAMBDA_INIT, scalar2=None, op0=ALU.mult),
             reads=[b_c], writes=[b_c])
        P.op("dve", lambda e: e.tensor_tensor(out=lt[:, 0, :], in0=l4[:, 0, :], in1=l4[:, 1, :], op=ALU.mult), reads=[b_c], writes=[b_c])
        P.op("dve", lambda e: e.tensor_tensor(out=lt[:, 1, :], in0=l4[:, 2, :], in1=l4[:, 3, :], op=ALU.mult), reads=[b_c], writes=[b_c])
        P.op("dve", lambda e: e.tensor_reduce(out=lam[:, 0:2], in_=lt[:], axis=AX.X, op=ALU.add), reads=[b_c], writes=[b_c])
        P.op("act", lambda e: e.activation(out=lam[:, 0:2], in_=lam[:, 0:2], func=AF.Exp), reads=[b_c], writes=[b_c])
        P.op("dve", lambda e: e.tensor_tensor(out=lam[:, 2:3], in0=lam[:, 1:2], in1=lam[:, 0:1], op=ALU.subtract), reads=[b_c], writes=[b_c])
        P.op("dve", lambda e: e.tensor_scalar(out=lam[:, 2:3], in0=lam[:, 2:3], scalar1=-LAMBDA_INIT, scalar2=None, op0=ALU.add),
             reads=[b_c], writes=[b_c])
        NSC = 5
        PT = [sb(nc, st, f"at_PT{i}", [128, 512], BF16) for i in range(NSC)]
        PTb = [Buf() for _ in range(NSC)]
        tmpb_t = [sb(nc, st, f"at_tmp{i}", [128, 512], F32) for i in range(2)]
        tmpb = [Buf() for _ in range(2)]
        daoT = [sb(nc, st, f"at_daoT{i}", [128, TOWN], BF16) for i in range(2)]
        daoTb = [Buf() for _ in range(2)]
        o0 = [sb(nc, st, f"at_o0{i}", [128, 128], F32) for i in range(2)]
        o0b = [Buf() for _ in range(2)]
        ob = [sb(nc, st, f"at_ob{i}", [128, 128], BF16) for i in range(2)]
        obb = [Buf() for _ in range(2)]
        rs = sb(nc, st, "at_rs", [128, 8], F32)
        b_rs = Buf()
        r0 = sb(nc, st, "at_r0", [128, 4], F32)
        r1 = sb(nc, st, "at_r1", [128, 4], F32)
        o_all = sb(nc, st, "at_oall", [128, 4, 128], F32)
        t_all = sb(nc, st, "at_tall", [128, 4, 128], F32)
        ob_all = sb(nc, st, "at_oball", [128, 4, 128], BF16)
        b_oall, b_tall, b_oball = Buf(), Buf(), Buf()
        junk = sb(nc, st, "at_junk", [128, 128], BF16)
        b_junk = Buf()
        accA = [C.ps[0], C.ps[1]]
        accB = C.ps[2]
        accb = [C.psb[0], C.psb[1], C.psb[2]]
        sps = [C.ps[3][:], C.ps[4][:], C.ps[5][:], C.pst[0][:].bitcast(F32), C.pst[1][:].bitcast(F32)]
        spsb = [C.psb[3], C.psb[4], C.psb[5], C.pstb[0], C.pstb[1]]
        n_s = 0
        n_pt = 0
        n_tmp = 0
        n_o = 0
        for h in range(8):
            s = h % 2
            P.dma("sp", KT[s][:], KT_d[h, :, :], writes=[hb[s]])
            P.dma("sp", QT[s][:], QT_d[h, :, :], writes=[hb[s]])
            P.dma("sp", V[s][:], V_d[h, :, :, :], writes=[hb[s]])
            P.dma("sp", Wb[s][:], abias[h, :, :], writes=[hb[s]])
            for qg in range(4):
                kb_near0 = 16 + qg * 4 - 1
                nkb = 16 + 4 * (qg + 1)
                def emit_qk_exp(kb, comp, h=h, qg=qg, s=s, kb_near0=kb_near0):
                    nonlocal n_s, n_pt, n_tmp
                    pr = slice(comp * 64, (comp + 1) * 64)
                    si = n_s % NSC
                    n_s += 1
                    P.op("pe", lambda e: e.matmul(
                        sps[si], lhsT=KT[s][pr, kb * 128:(kb + 1) * 128], rhs=QT[s][pr, qg * 512:(qg + 1) * 512],
                        start=True, stop=True), reads=[hb[s]], writes=[spsb[si]])
                    pi = n_pt % NSC
                    n_pt += 1
                    if kb < kb_near0:
                        fcol = 2 * h + (0 if kb < 16 else 1)
                        P.op("act", lambda e: e.activation(
                            out=PT[pi][:], in_=sps[si], func=AF.Exp, bias=fb[:, fcol:fcol + 1]),
                            reads=[spsb[si], b_c], writes=[PTb[pi]])
                    else:
                        j = kb - kb_near0
                        u0 = 512 - 128 * j
                        ti = n_tmp % 2
                        n_tmp += 1
                        if kb < 16:
                            P.op("dve", lambda e: e.scalar_tensor_tensor(
                                out=tmpb_t[ti][:], in0=sps[si], scalar=fb[:, 16:17], in1=Wb[s][:, u0:u0 + 512],
                                op0=ALU.add, op1=ALU.add), reads=[spsb[si], hb[s], b_c], writes=[tmpb[ti]])
                        else:
                            P.op("dve", lambda e: e.tensor_tensor(
                                out=tmpb_t[ti][:], in0=sps[si], in1=Wb[s][:, u0:u0 + 512], op=ALU.add),
                                reads=[spsb[si], hb[s]], writes=[tmpb[ti]])
                        P.op("act", lambda e: e.activation(out=PT[pi][:], in_=tmpb_t[ti][:], func=AF.Exp),
                             reads=[tmpb[ti]], writes=[PTb[pi]])
                    return pi

                def emit_pv(kb, comp, pi, s=s, nkb=nkb):
                    def pv(e):
                        ins = None
                        for sub in range(int(os.environ.get("ATT_PVSUBS", "4"))):
                            if sub < 3:
                                out = accA[comp][:, sub * 129:(sub + 1) * 129]
                                first = (kb == 0 and sub == 0)
                            else:
                                out = accB[:, comp * 129:(comp + 1) * 129]
                                first = (kb == 0 and comp == 0)
                            ins = e.matmul(out, lhsT=PT[pi][:, sub * 128:(sub + 1) * 128], rhs=V[s][:, kb, :],
                                           start=first, stop=(kb == nkb - 1), skip_group_check=True)
                        return ins
                    P.op("pe", pv, reads=[PTb[pi], hb[s]], writes=[accb[comp], accb[2]])

                its = [(kb, comp) for kb in range(nkb) for comp in range(2)]
                LA = 3
                pend = []
                for (kb, comp) in its:
                    pi = emit_qk_exp(kb, comp)
                    pend.append((kb, comp, pi))
                    if len(pend) > LA:
                        emit_pv(*pend.pop(0))
                while pend:
                    emit_pv(*pend.pop(0))
                if C.debug is not None and C.debug[0] == "attn_acc" and (h, qg) == (0, 0):
                    P.barrier()
                    for bi in range(3):
                        P.op("dve", lambda e, bi=bi: e.tensor_copy(out=tmpb_t[0][:], in_=C.ps[bi][:]), reads=[accb[bi]], writes=[tmpb[0]])
                        P.dma("sp", C.dbg[bi], tmpb_t[0][:], reads=[tmpb[0]], is_out=True)
                    P.finish()
                    C.stopped = True
                    return
                pt_o, ptb_o = next_pst(C)
                A0 = accA[0][:, 0:387].rearrange("p (s c) -> p s c", c=129)
                A1 = accA[1][:, 0:387].rearrange("p (s c) -> p s c", c=129)
                B2 = accB[:, 0:258].rearrange("p (s c) -> p s c", c=129)
                Fd = lambda fn, r=(), w=(): P.op("dve", fn, reads=list(r), writes=list(w))
                Fd(lambda e: e.reciprocal(out=r0[:, 0:3], in_=A0[:, :, 128]), [accb[0]], [b_rs])
                Fd(lambda e: e.reciprocal(out=r0[:, 3:4], in_=B2[:, 0, 128:129]), [accb[2]], [b_rs])
                Fd(lambda e: e.reciprocal(out=r1[:, 0:3], in_=A1[:, :, 128]), [accb[1]], [b_rs])
                Fd(lambda e: e.reciprocal(out=r1[:, 3:4], in_=B2[:, 1, 128:129]), [accb[2]], [b_rs])
                Fd(lambda e: e.tensor_scalar(out=r1[:], in0=r1[:], scalar1=lam[:, 2:3], scalar2=None, op0=ALU.mult), [b_c], [b_rs])
                Fd(lambda e: e.tensor_tensor(out=o_all[:, 0:3, :], in0=A0[:, :, 0:128],
                                             in1=r0[:, 0:3].unsqueeze(2).to_broadcast([128, 3, 128]), op=ALU.mult), [accb[0], b_rs], [b_oall])
                Fd(lambda e: e.tensor_scalar(out=o_all[:, 3, :], in0=B2[:, 0, 0:128], scalar1=r0[:, 3:4], scalar2=None, op0=ALU.mult),
                   [accb[2], b_rs], [b_oall])
                Fd(lambda e: e.tensor_tensor(out=t_all[:, 0:3, :], in0=A1[:, :, 0:128],
                                             in1=r1[:, 0:3].unsqueeze(2).to_broadcast([128, 3, 128]), op=ALU.mult), [accb[1], b_rs], [b_tall])
                Fd(lambda e: e.tensor_scalar(out=t_all[:, 3, :], in0=B2[:, 1, 0:128], scalar1=r1[:, 3:4], scalar2=None, op0=ALU.mult),
                   [accb[2], b_rs], [b_tall])
                Fd(lambda e: e.tensor_tensor(out=o_all[:], in0=o_all[:], in1=t_all[:], op=ALU.add), [b_tall], [b_oall])
                Fd(lambda e: e.tensor_tensor(out=t_all[:], in0=o_all[:], in1=o_all[:], op=ALU.mult), [b_oall], [b_tall])
                Fd(lambda e: e.tensor_reduce(out=rs[:, 0:4], in_=t_all[:], axis=AX.X, op=ALU.add), [b_tall], [b_rs])
                P.op("act", lambda e: e.activation(out=rs[:, 4:8], in_=rs[:, 0:4], func=AF.Ln, scale=1.0 / 128, bias=C.cst[:, 0:1]),
                     reads=[C.b_cst], writes=[b_rs])
                P.op("act", lambda e: e.activation(out=rs[:, 4:8], in_=rs[:, 4:8], func=AF.Exp, scale=-0.5), reads=[], writes=[b_rs])
                Fd(lambda e: e.tensor_tensor(out=t_all[:], in0=o_all[:], in1=rs[:, 4:8].unsqueeze(2).to_broadcast([128, 4, 128]), op=ALU.mult),
                   [b_oall, b_rs], [b_tall])
                Fd(lambda e: e.tensor_tensor(out=ob_all[:], in0=t_all[:], in1=dag[:].unsqueeze(1).to_broadcast([128, 4, 128]), op=ALU.mult),
                   [b_tall, b_c], [b_oball])

                def tr4(e, pt_o=pt_o):
                    ins = None
                    for sub in range(4):
                        ins = e.transpose(out=pt_o[:, sub * 128:(sub + 1) * 128], in_=ob_all[:, sub, :], identity=C.ident[:])
                    return ins
                P.op("pe", tr4, reads=[b_oball, C.b_ident], writes=[ptb_o])
                P.barrier()
                P.op("act", lambda e, pt_o=pt_o, qg=qg, s=s: e.copy(out=daoT[s][:, qg * 512:(qg + 1) * 512], in_=pt_o[:, 0:512]),
                     reads=[ptb_o], writes=[daoTb[s]])
            P.dma("sp", daoT_d[:, h, :], daoT[s][:], reads=[daoTb[s]])
        P.barrier()


def load_rows_w(C, slot, slotb, w_ap, nk, col0, ncol):
    wv = w_ap.rearrange("(c p) n -> p c n", p=128)
    C.P.dma("pool", slot[:, 0:nk, 0:ncol], wv[:, :, col0:col0 + ncol], writes=[slotb])


def phase4_mix_out(C):
    nc, P = C.nc, C.P
    hgoT_d = C.dscr("hgoT", [128, 8, TOWN], BF16)
    daoT_d = C.dscr("daoT", [128, 8, TOWN], BF16)
    SG_d = C.dscr("SG", [2, 16, 128, TOWN], BF16)
    x1_d = C.dscr("x1", [TOWN, D], F32)
    xw = C.din("xw", [TWIN, D])
    wa = C.din("w_branch_a", [1024, D])
    wb_ = C.din("w_branch_b", [1024, D])
    wo = C.din("w_out", [D, D])
    with ExitStack() as st:
        hgoT = sb(nc, st, "p4_hgoT", [128, 8, TOWN], BF16)
        daoT = sb(nc, st, "p4_daoT", [128, 8, TOWN], BF16)
        mT = sb(nc, st, "p4_mT", [128, NCH, TOWN], BF16)
        b_in = Buf()
        mTb = [Buf() for _ in range(4)]
        P.dma("sp", hgoT[:], hgoT_d[:, :, :], writes=[b_in])
        P.dma("sp", daoT[:], daoT_d[:, :, :], writes=[b_in])
        with ExitStack() as s2:
            was = [sb(nc, s2, f"p4_wa{i}", [128, 8, 512], BF16) for i in range(2)]
            wbs = [sb(nc, s2, f"p4_wb{i}", [128, 8, 512], BF16) for i in range(2)]
            wab = [Buf() for _ in range(2)]
            wbb = [Buf() for _ in range(2)]
            sga = [sb(nc, s2, f"p4_sga{i}", [128, TOWN], BF16) for i in range(2)]
            sgb_ = [sb(nc, s2, f"p4_sgb{i}", [128, TOWN], BF16) for i in range(2)]
            sgab = [Buf() for _ in range(2)]
            sgbb = [Buf() for _ in range(2)]
            t1 = [sb(nc, s2, f"p4_t1{i}", [128, 512], F32) for i in range(2)]
            t2 = [sb(nc, s2, f"p4_t2{i}", [128, 512], F32) for i in range(2)]
            t1b = [Buf() for _ in range(2)]
            t2b = [Buf() for _ in range(2)]
            n = 0
            for cg in range(4):
                ws = cg % 2
                load_rows_w(C, was[ws], wab[ws], wa, 8, cg * 512, 512)
                load_rows_w(C, wbs[ws], wbb[ws], wb_, 8, cg * 512, 512)
                for cc in range(4):
                    ch = cg * 4 + cc
                    gs = ch % 2
                    P.dma("sp", sga[gs][:], SG_d[0, ch, :, :], writes=[sgab[gs]])
                    P.dma("sp", sgb_[gs][:], SG_d[1, ch, :, :], writes=[sgbb[gs]])
                    for tg in range(4):
                        sl = slice(tg * 512, (tg + 1) * 512)
                        ti = n % 2
                        n += 1
                        for (wsl_, wslb_, src, tt, ttb, sg_, sgb2) in ((was[ws], wab[ws], hgoT, t1, t1b, sga[gs], sgab[gs]),
                                                                      (wbs[ws], wbb[ws], daoT, t2, t2b, sgb_[gs], sgbb[gs])):
                            ps, psb = next_ps(C)

                            def mm(e, ps=ps, wsl_=wsl_, src=src, cc=cc, sl=sl):
                                ins = None
                                for k in range(8):
                                    ins = e.matmul(ps[:], lhsT=wsl_[:, k, cc * 128:(cc + 1) * 128], rhs=src[:, k, sl],
                                                   start=(k == 0), stop=(k == 7))
                                return ins
                            P.op("pe", mm, reads=[wslb_, b_in], writes=[psb])
                            P.op("dve", lambda e, ps=ps, tt=tt, ti=ti, sg_=sg_, sl=sl: e.tensor_tensor(
                                out=tt[ti][:], in0=ps[:], in1=sg_[:, sl], op=ALU.mult), reads=[psb, sgb2], writes=[ttb[ti]])
                        P.op("dve", lambda e, ti=ti, ch=ch, sl=sl: e.tensor_tensor(
                            out=mT[:, ch, sl], in0=t1[ti][:], in1=t2[ti][:], op=ALU.add),
                            reads=[t1b[ti], t2b[ti]], writes=[mTb[tg]])
            P.barrier()
        if C.debug is not None and C.debug[0] == "mT":
            P.barrier()
            P.dma("sp", C.dbg, mT[:], is_out=True)
            P.finish()
            C.stopped = True
            return
        with ExitStack() as s2:
            wos = [sb(nc, s2, f"p4_wo{i}", [128, NCH, 512], BF16) for i in range(2)]
            wob = [Buf() for _ in range(2)]
            g1 = sb(nc, s2, "p4_g1", [128, D], F32)
            b_g1 = Buf()
            P.dma("sp", g1[:], C.mod_d[0:1, 2 * D:3 * D].partition_broadcast(128), writes=[b_g1])
            xt = [sb(nc, s2, f"p4_xt{i}", [128, 512], F32) for i in range(3)]
            xtb = [Buf() for _ in range(3)]
            yt = [sb(nc, s2, f"p4_yt{i}", [128, 512], F32) for i in range(2)]
            ytb = [Buf() for _ in range(2)]
            n = 0
            for cg in range(4):
                ws = cg % 2
                load_rows_w(C, wos[ws], wob[ws], wo, NCH, cg * 512, 512)
                csl = slice(cg * 512, (cg + 1) * 512)
                for tt in range(16):
                    xi = n % 3
                    yi = n % 2
                    n += 1
                    P.dma("sp", xt[xi][:], xw[TOWN + tt * 128:TOWN + (tt + 1) * 128, csl], writes=[xtb[xi]])
                    ps, psb = next_ps(C)

                    def mm(e, ps=ps, ws=ws, tt=tt):
                        ins = None
                        for k in range(NCH):
                            ins = e.matmul(ps[:], lhsT=mT[:, k, tt * 128:(tt + 1) * 128], rhs=wos[ws][:, k, :],
                                           start=(k == 0), stop=(k == NCH - 1))
                        return ins
                    P.op("pe", mm, reads=[wob[ws], mTb[tt // 4]], writes=[psb])
                    P.op("dve", lambda e, ps=ps, yi=yi, csl=csl: e.tensor_tensor(out=yt[yi][:], in0=ps[:], in1=g1[:, csl], op=ALU.mult),
                         reads=[psb, b_g1], writes=[ytb[yi]])
                    P.op("dve", lambda e, yi=yi, xi=xi: e.tensor_tensor(out=xt[xi][:], in0=yt[yi][:], in1=xt[xi][:], op=ALU.add),
                         reads=[ytb[yi]], writes=[xtb[xi]])
                    P.dma("sp", x1_d[tt * 128:(tt + 1) * 128, csl], xt[xi][:], reads=[xtb[xi]])
            P.barrier()
        P.barrier()


CAP = 1024
NSLOT = 64 * CAP
BIG = 1.0e9


def phase5_route(C, st_keep):
    nc, P = C.nc, C.P
    x1_d = C.dscr("x1", [TOWN, D], F32)
    Xs_d = C.dscr("Xs", [NSLOT, D], BF16)
    Y_d = C.dscr("Y", [NSLOT, D], BF16)
    Ysh_d = C.dscr("Ysh", [TOWN, D], BF16)
    norm2_g = C.din("norm2_g", [1, D])
    C.bc_reg = nc.gpsimd.to_reg(NSLOT - 1)
    C.flag_i = sb(nc, st_keep, "flag_i", [1, 512], I32)
    C.idx_all = sb(nc, st_keep, "idx_all", [128, 128], I32)
    C.w_all = sb(nc, st_keep, "w_all", [128, 128], F32)
    C.b_idx = [Buf() for _ in range(16)]
    with ExitStack() as st:
        G = sb(nc, st, "n2_G", [128, D], F32)
        S = sb(nc, st, "n2_S", [128, D], F32)
        t0 = sb(nc, st, "n2_t0", [128, D], F32)
        bG, bS, bt0 = Buf(), Buf(), Buf()
        P.dma("sp", G[:], C.mod_d[0:1, 4 * D:5 * D].partition_broadcast(128), writes=[bG])
        P.dma("sp", t0[:], norm2_g[0:1, :].partition_broadcast(128), writes=[bt0])
        P.dma("sp", S[:], C.mod_d[0:1, 3 * D:4 * D].partition_broadcast(128), writes=[bS])
        P.op("dve", lambda e: e.scalar_tensor_tensor(out=G[:], in0=G[:], scalar=1.0, in1=t0[:], op0=ALU.add, op1=ALU.mult),
             reads=[bt0], writes=[bG])
        h2f, b_h2f = t0, bt0
        hTs = sb(nc, st, "p5_hTs", [128, NCH, 256], BF16)
        _hslots = [Buf(), Buf()]
        hTsb = [_hslots[i % 2] for i in range(16)]
        h2T32 = sb(nc, st, "p5_h2T32", [128, NCH, 128], F32)
        b_h2T32 = Buf()
        wr = sb(nc, st, "p5_wr", [128, NCH, 64], F32)
        rb_bc = sb(nc, st, "p5_rb", [128, 64], F32)
        ecap = sb(nc, st, "p5_ecap", [128, 64], F32)
        ustr_f = sb(nc, st, "p5_ustr_f", [128, 128], F32)
        ustr = sb(nc, st, "p5_ustr", [128, 128], BF16)
        ones_b = sb(nc, st, "p5_ones", [128, 128], BF16)
        base = sb(nc, st, "p5_base", [128, 64], F32)
        b_c, b_base = Buf(), Buf()
        P.dma("sp", wr[:], C.din("router_w", [D, 64]).rearrange("(c p) n -> p c n", p=128), writes=[b_c])
        P.dma("sp", rb_bc[:], C.din("router_bias", [1, 64])[0:1, :].partition_broadcast(128), writes=[b_c])
        P.dma("sp", ecap[:], C.din("ecap", [1, 64])[0:1, :].partition_broadcast(128), writes=[b_c])
        P.dma("sp", ustr_f[:], C.din("ustrict", [128, 128])[:, :], writes=[b_c])
        P.op("dve", lambda e: e.tensor_copy(out=ustr[:], in_=ustr_f[:]), reads=[b_c], writes=[b_c])
        P.op("dve", lambda e: e.memset(ones_b[:], 1.0), writes=[b_c])
        P.op("dve", lambda e: e.memset(base[:], 0.0), writes=[b_base])
        wsg = sb(nc, st, "p5_wsg", [128, NCH, 512], BF16)
        wsu = sb(nc, st, "p5_wsu", [128, NCH, 512], BF16)
        wsd = sb(nc, st, "p5_wsd", [128, 4, D], BF16)
        b_ws = Buf()
        load_rows_w(C, wsg, b_ws, C.din("w_sh_gate", [D, 512]), NCH, 0, 512)
        load_rows_w(C, wsu, b_ws, C.din("w_sh_up", [D, 512]), NCH, 0, 512)
        load_rows_w(C, wsd, b_ws, C.din("w_sh_down", [512, D]), 4, 0, D)
        def t(name, shape, dt=F32):
            return sb(nc, st, "p5_" + name, shape, dt)
        sc = t("sc", [128, 64]); sel = t("sel", [128, 64]); eq = t("eq", [128, 64]); sel2 = t("sel2", [128, 64])
        m1 = t("m1", [128, 8]); m2 = t("m2", [128, 8]); gs = t("gs", [128, 8]); t8 = t("t8", [128, 8])
        gm = t("gm", [128, 8]); pen = t("pen", [128, 8]); selm = t("selm", [128, 64]); em = t("em", [128, 64])
        emb = t("emb", [128, 64], BF16); gw = t("gw", [128, 64]); den = t("den", [128, 2]); Gt = t("Gt", [128, 64])
        pos = t("pos", [128, 64]); val = t("val", [128, 64]); v01 = t("v01", [128, 64]); top8 = t("top8", [128, 8])
        neg = t("neg", [128, 8]); idxf = t("idxf", [128, 8]); oh = t("oh", [128, 64]); ohj = t("ohj", [128, 64])
        b_r = Buf()
        sgs = t("sgs", [128, 128]); h1s = t("h1s", [128, 4, 128], BF16); ysh = [t(f"ysh{i}", [128, D], BF16) for i in range(2)]
        b_sgs, b_h1s = Buf(), Buf()
        yshb = [Buf() for _ in range(2)]

        def h32_cb(i, tmp, btmp, S_, bS_, hb_s, hbb_s):
            P.op("dve", lambda e: e.tensor_tensor(out=h2f[:], in0=tmp[:], in1=S_[:], op=ALU.add),
                 reads=[btmp, bS_], writes=[b_h2f])
            P.op("act", lambda e: e.copy(out=hb_s[:], in_=h2f[:]), reads=[b_h2f], writes=[hbb_s])
            for q4 in range(4):
                ps, psb = next_ps(C)

                def tr(e, ps=ps, q4=q4):
                    ins = None
                    for c in range(4):
                        cc = q4 * 4 + c
                        ins = e.transpose(out=ps[:, c * 128:(c + 1) * 128], in_=h2f[:, cc * 128:(cc + 1) * 128],
                                          identity=C.ident_f[:])
                    return ins
                P.op("pe", tr, reads=[b_h2f, C.b_ident], writes=[psb])
                P.op("act", lambda e, ps=ps, q4=q4: e.copy(out=h2T32[:, q4 * 4:(q4 + 1) * 4, :],
                                                           in_=ps[:].rearrange("p (c t) -> p c t", t=128)),
                     reads=[psb], writes=[b_h2T32])
            ps, psb = next_ps(C)

            def mmr(e, ps=ps):
                ins = None
                for c in range(NCH):
                    ins = e.matmul(ps[:, 0:64], lhsT=h2T32[:, c, :], rhs=wr[:, c, :], start=(c == 0), stop=(c == NCH - 1))
                return ins
            P.op("pe", mmr, reads=[b_h2T32, b_c], writes=[psb])
            D_ = lambda fn, extra=(): P.op("dve", fn, reads=[b_c] + list(extra), writes=[b_r])
            P.op("act", lambda e, ps=ps: e.activation(out=sc[:], in_=ps[:, 0:64], func=AF.Sigmoid), reads=[psb], writes=[b_r])
            D_(lambda e: e.tensor_tensor(out=sel[:], in0=sc[:], in1=rb_bc[:], op=ALU.add))
            g3 = lambda ap: ap.rearrange("p (g k) -> p g k", k=8)
            D_(lambda e: e.tensor_reduce(out=m1[:], in_=g3(sel[:]), axis=AX.X, op=ALU.max))
            D_(lambda e: e.tensor_tensor(out=g3(eq[:]), in0=g3(sel[:]), in1=m1[:].unsqueeze(2).to_broadcast([128, 8, 8]), op=ALU.is_equal))
            D_(lambda e: e.scalar_tensor_tensor(out=sel2[:], in0=eq[:], scalar=-BIG, in1=sel[:], op0=ALU.mult, op1=ALU.add))
            D_(lambda e: e.tensor_reduce(out=m2[:], in_=g3(sel2[:]), axis=AX.X, op=ALU.max))
            D_(lambda e: e.tensor_tensor(out=gs[:], in0=m1[:], in1=m2[:], op=ALU.add))
            D_(lambda e: e.max(out=t8[:], in_=gs[:]))
            D_(lambda e: e.tensor_scalar(out=gm[:], in0=gs[:], scalar1=t8[:, 3:4], scalar2=None, op0=ALU.is_ge))
            D_(lambda e: e.tensor_scalar(out=pen[:], in0=gm[:], scalar1=BIG, scalar2=-BIG, op0=ALU.mult, op1=ALU.add))
            D_(lambda e: e.tensor_tensor(out=g3(selm[:]), in0=g3(sel[:]), in1=gm[:].unsqueeze(2).to_broadcast([128, 8, 8]), op=ALU.mult))
            D_(lambda e: e.tensor_tensor(out=g3(selm[:]), in0=g3(selm[:]), in1=pen[:].unsqueeze(2).to_broadcast([128, 8, 8]), op=ALU.add))
            D_(lambda e: e.max(out=t8[:], in_=selm[:]))
            D_(lambda e: e.tensor_scalar(out=em[:], in0=selm[:], scalar1=t8[:, 7:8], scalar2=None, op0=ALU.is_ge))
            D_(lambda e: e.tensor_copy(out=emb[:], in_=em[:]))
            D_(lambda e: e.tensor_tensor(out=gw[:], in0=sc[:], in1=em[:], op=ALU.mult))
            D_(lambda e: e.tensor_reduce(out=den[:, 0:1], in_=gw[:], axis=AX.X, op=ALU.add))
            D_(lambda e: e.reciprocal(out=den[:, 1:2], in_=den[:, 0:1]))
            D_(lambda e: e.tensor_scalar(out=Gt[:], in0=gw[:], scalar1=den[:, 1:2], scalar2=2.5, op0=ALU.mult, op1=ALU.mult))
            psp, pspb = next_ps(C)
            P.op("pe", lambda e, psp=psp: e.matmul(psp[:, 0:64], lhsT=ustr[:], rhs=emb[:], start=True, stop=True),
                 reads=[b_r, b_c], writes=[pspb])
            P.op("dve", lambda e, psp=psp: e.tensor_tensor(out=pos[:], in0=psp[:, 0:64], in1=base[:], op=ALU.add),
                 reads=[pspb, b_base], writes=[b_r])
            pst_, pstb_ = next_ps(C)
            P.op("pe", lambda e, pst_=pst_: e.matmul(pst_[:, 0:64], lhsT=ones_b[:], rhs=emb[:], start=True, stop=True),
                 reads=[b_r, b_c], writes=[pstb_])
            P.op("dve", lambda e, pst_=pst_: e.tensor_tensor(out=base[:], in0=pst_[:, 0:64], in1=base[:], op=ALU.add),
                 reads=[pstb_, b_r], writes=[b_base])
            D_(lambda e: e.tensor_scalar(out=v01[:], in0=pos[:], scalar1=float(CAP), scalar2=None, op0=ALU.is_lt))
            D_(lambda e: e.tensor_tensor(out=v01[:], in0=v01[:], in1=em[:], op=ALU.mult))
            D_(lambda e: e.scalar_tensor_tensor(out=val[:], in0=pos[:], scalar=1.0, in1=ecap[:], op0=ALU.add, op1=ALU.add))
            D_(lambda e: e.tensor_tensor(out=val[:], in0=val[:], in1=v01[:], op=ALU.mult))
            D_(lambda e: e.tensor_scalar(out=val[:], in0=val[:], scalar1=-1.0, scalar2=None, op0=ALU.add))
            D_(lambda e: e.max(out=top8[:], in_=val[:]))
            D_(lambda e: e.tensor_scalar(out=neg[:], in0=top8[:], scalar1=0.0, scalar2=None, op0=ALU.is_lt))
            D_(lambda e: e.scalar_tensor_tensor(out=idxf[:], in0=neg[:], scalar=4.0e6, in1=top8[:], op0=ALU.mult, op1=ALU.add))
            P.op("dve", lambda e: e.tensor_copy(out=C.idx_all[:, i * 8:(i + 1) * 8], in_=idxf[:]), reads=[b_r], writes=[C.b_idx[i]])
            for k in range(8):
                D_(lambda e, k=k: e.tensor_scalar(out=oh[:], in0=val[:], scalar1=top8[:, k:k + 1], scalar2=None, op0=ALU.is_equal))
                D_(lambda e: e.tensor_tensor(out=ohj[:], in0=oh[:], in1=Gt[:], op=ALU.mult))
                P.op("dve", lambda e, k=k: e.tensor_reduce(out=C.w_all[:, i * 8 + k:i * 8 + k + 1], in_=ohj[:], axis=AX.X, op=ALU.add),
                     reads=[b_r], writes=[C.b_idx[i]])
            D_(lambda e: e.tensor_scalar(out=neg[:], in0=neg[:], scalar1=-1.0, scalar2=1.0, op0=ALU.mult, op1=ALU.add))
            P.op("dve", lambda e: e.tensor_tensor(out=C.w_all[:, i * 8:(i + 1) * 8], in0=C.w_all[:, i * 8:(i + 1) * 8], in1=neg[:], op=ALU.mult),
                 reads=[b_r], writes=[C.b_idx[i]])
            if os.environ.get("IND_PROBE") and i == 0:
                with ExitStack() as sp_:
                    it_ = sb(nc, sp_, "prb_it", [128, 128], I32)
                    xb_ = sb(nc, sp_, "prb_xb", [128, D], BF16)
                    sem_ = P.q["pool"].ring[0].sem
                    for nm, idx_ap, in_ap in (("fresh/fresh", it_[:, 3:4], xb_[:, :]), ("idx_all/fresh", C.idx_all[:, 3:4], xb_[:, :]),
                                              ("fresh/hb", it_[:, 3:4], hb_s[:, :]), ("idx_all/hb", C.idx_all[:, 0:1], hb_s[:, :])):
                        try:
                            nc.gpsimd.indirect_dma_start(out=Xs_d[:, :], out_offset=bass.IndirectOffsetOnAxis(ap=idx_ap, axis=0),
                                                         in_=in_ap, in_offset=None, bounds_check=C.bc_reg, oob_is_err=False).then_inc(sem_, 16)
                            print("IND_PROBE ok", nm)
                        except Exception as ex:
                            print("IND_PROBE fail", nm, ex)
            for k in range(8):
                if os.environ.get("IND_PROBE"):
                    print("IND_PROBE emitting scatter", i, k, flush=True)
                P.dma_custom("pool", lambda e, k=k: e.indirect_dma_start(
                    out=Xs_d[:, :], out_offset=bass.IndirectOffsetOnAxis(ap=C.idx_all[:, i * 8 + k:i * 8 + k + 1], axis=0),
                    in_=hb_s[:, :], in_offset=None, bounds_check=C.bc_reg, oob_is_err=False),
                    reads=[C.b_idx[i], hbb_s])

        def post_cb(i, hb_s, hbb_s, c0):
            for fc in range(4):
                psg, psgb = next_ps(C)
                psu, psub = next_ps(C)
                for (ps_, psb_, w_) in ((psg, psgb, wsg), (psu, psub, wsu)):
                    def mm(e, ps_=ps_, w_=w_, fc=fc):
                        ins = None
                        for c in range(NCH):
                            ins = e.matmul(ps_[:, 0:128], lhsT=w_[:, c, fc * 128:(fc + 1) * 128], rhs=hTs[:, c, c0:c0 + 128],
                                           start=(c == 0), stop=(c == NCH - 1))
                        return ins
                    P.op("pe", mm, reads=[b_ws, hTsb[i]], writes=[psb_])
                P.op("act", lambda e, psg=psg: e.activation(out=sgs[:], in_=psg[:, 0:128], func=AF.Silu), reads=[psgb], writes=[b_sgs])
                P.op("dve", lambda e, psu=psu, fc=fc: e.tensor_tensor(out=h1s[:, fc, :], in0=sgs[:], in1=psu[:, 0:128], op=ALU.mult),
                     reads=[b_sgs, psub], writes=[b_h1s])
            yi = i % 2
            for cg in range(4):
                ps, psb = next_ps(C)

                def mm(e, ps=ps, cg=cg):
                    ins = None
                    for fc in range(4):
                        ins = e.matmul(ps[:], lhsT=h1s[:, fc, :], rhs=wsd[:, fc, cg * 512:(cg + 1) * 512],
                                       start=(fc == 0), stop=(fc == 3))
                    return ins
                P.op("pe", mm, reads=[b_h1s, b_ws], writes=[psb])
                P.op("act", lambda e, ps=ps, cg=cg, yi=yi: e.copy(out=ysh[yi][:, cg * 512:(cg + 1) * 512], in_=ps[:]),
                     reads=[psb], writes=[yshb[yi]])
            P.dma("sp", Ysh_d[i * 128:(i + 1) * 128, :], ysh[yi][:], reads=[yshb[yi]])

        norm_tiles(C, st, lambda i: x1_d[i * 128:(i + 1) * 128, :], G, bG, S, bS, hTs, hTsb, "n2",
                   h32_cb=h32_cb, col_fn=lambda i: (i % 2) * 128, post_cb=post_cb)
        thr = sb(nc, st, "p5_thr", [1, 512], F32)
        flf = sb(nc, st, "p5_flf", [1, 512], F32)
        b_f = Buf()
        P.dma("sp", thr[:], C.din("thr8", [1, 512])[:, :], writes=[b_f])
        P.op("dve", lambda e: e.tensor_tensor(out=flf[:].rearrange("p (e b) -> p e b", b=8),
                                              in0=base[0:1, :].unsqueeze(2).to_broadcast([1, 64, 8]),
                                              in1=thr[:].rearrange("p (e b) -> p e b", b=8), op=ALU.is_gt),
             reads=[b_base, b_f], writes=[b_f])
        P.op("dve", lambda e: e.tensor_copy(out=C.flag_i[:], in_=flf[:]), reads=[b_f], writes=[b_f])
        P.barrier()


def phase6_experts(C):
    nc, P = C.nc, C.P
    Xs_d = C.dscr("Xs", [NSLOT, D], BF16)
    Y_d = C.dscr("Y", [NSLOT, D], BF16)
    Ysh_d = C.dscr("Ysh", [TOWN, D], BF16)
    weg = C.din("w_exp_gate", [64, D, 512])
    weu = C.din("w_exp_up", [64, D, 512])
    wed = C.din("w_exp_down", [64, 512, D])
    with ExitStack() as st:
        wg = [sb(nc, st, f"p6_wg{i}", [128, NCH, 512], BF16) for i in range(2)]
        wu = [sb(nc, st, f"p6_wu{i}", [128, NCH, 512], BF16) for i in range(2)]
        wd = [sb(nc, st, f"p6_wd{i}", [128, 4, D], BF16) for i in range(2)]
        wb = [Buf() for _ in range(2)]
        xtok = [sb(nc, st, f"p6_xtok{i}", [128, D], BF16) for i in range(3)]
        xtokb = [Buf() for _ in range(3)]
        XT = [sb(nc, st, f"p6_XT{i}", [128, NCH, 512], BF16) for i in range(2)]
        XTb = [Buf() for _ in range(2)]
        sg = [sb(nc, st, f"p6_sg{i}", [128, 512], F32) for i in range(2)]
        sgb = [Buf() for _ in range(2)]
        h1 = [sb(nc, st, f"p6_h1{i}", [128, 4, 512], BF16) for i in range(2)]
        h1b = [Buf() for _ in range(2)]
        yev = [sb(nc, st, f"p6_yev{i}", [128, D], BF16) for i in range(2)]
        yevb = [Buf() for _ in range(2)]
        n_x = 0
        n_g = 0
        n_y = 0
        n_sg = 0
        NG = CAP // 512
        for e in range(int(os.environ.get("P6_NEXP", "64"))):
            ws = e % 2
            if not (os.environ.get("P6_NOW") and e >= 2):
                load_rows_w(C, wg[ws], wb[ws], weg[e], NCH, 0, 512)
                load_rows_w(C, wu[ws], wb[ws], weu[e], NCH, 0, 512)
                load_rows_w(C, wd[ws], wb[ws], wed[e], 4, 0, D)
            for g2 in range(NG):
                gi = n_g % 2
                n_g += 1
                slot0 = e * CAP + g2 * 512
                for blk in range(4):
                    xi = n_x % 3
                    n_x += 1
                    r0 = slot0 + blk * 128
                    P.dma_cond(C.flag_i[0:1, e * 8 + g2 * 4 + blk:e * 8 + g2 * 4 + blk + 1], xtok[xi][:], Xs_d[r0:r0 + 128, :],
                               writes=[xtokb[xi]])
                    for hlf in range(2):
                        pt, ptb = next_pst(C)

                        def tr(e_, xi=xi, hlf=hlf, pt=pt):
                            ins = None
                            for c in range(8):
                                cc = hlf * 8 + c
                                ins = e_.transpose(out=pt[:, c * 128:(c + 1) * 128], in_=xtok[xi][:, cc * 128:(cc + 1) * 128],
                                                   identity=C.ident[:])
                            return ins
                        P.op_cond(C.flag_i[0:1, e * 8 + g2 * 4 + blk:e * 8 + g2 * 4 + blk + 1], tr,
                                  reads=[xtokb[xi], C.b_ident], writes=[ptb])
                        eng = "act" if hlf == 0 else "dve"
                        if eng == "act":
                            P.op_cond(C.flag_i[0:1, e * 8 + g2 * 4 + blk:e * 8 + g2 * 4 + blk + 1], lambda e_, gi=gi, hlf=hlf, blk=blk, pt=pt: e_.copy(
                                out=XT[gi][:, hlf * 8:(hlf + 1) * 8, blk * 128:(blk + 1) * 128],
                                in_=pt[:].rearrange("p (c t) -> p c t", t=128)), reads=[ptb], writes=[XTb[gi]], qname="act")
                        else:
                            P.op_cond(C.flag_i[0:1, e * 8 + g2 * 4 + blk:e * 8 + g2 * 4 + blk + 1], lambda e_, gi=gi, hlf=hlf, blk=blk, pt=pt: e_.tensor_copy(
                                out=XT[gi][:, hlf * 8:(hlf + 1) * 8, blk * 128:(blk + 1) * 128],
                                in_=pt[:].rearrange("p (c t) -> p c t", t=128)), reads=[ptb], writes=[XTb[gi]], qname="dve")
                for fc in range(4):
                    psg, psgb = next_ps(C)
                    psu, psub = next_ps(C)

                    def mm(e_, psg=psg, psu=psu, fc=fc, gi=gi, ws=ws):
                        ins = None
                        for (ps_, w_) in ((psg, wg[ws]), (psu, wu[ws])):
                            for c in range(NCH):
                                ins = e_.matmul(ps_[:], lhsT=w_[:, c, fc * 128:(fc + 1) * 128], rhs=XT[gi][:, c, :],
                                                start=(c == 0), stop=(c == NCH - 1))
                        return ins
                    P.op_cond(C.flag_i[0:1, e * 8 + g2 * 4:e * 8 + g2 * 4 + 1], mm, reads=[wb[ws], XTb[gi]], writes=[psgb, psub])
                    si = n_sg % 2
                    n_sg += 1
                    P.op_cond(C.flag_i[0:1, e * 8 + g2 * 4:e * 8 + g2 * 4 + 1], lambda e_, psg=psg, si=si: e_.activation(out=sg[si][:], in_=psg[:], func=AF.Silu),
                              reads=[psgb], writes=[sgb[si]], qname="act")
                    P.op_cond(C.flag_i[0:1, e * 8 + g2 * 4:e * 8 + g2 * 4 + 1], lambda e_, psu=psu, si=si, gi=gi, fc=fc: e_.tensor_tensor(
                        out=h1[gi][:, fc, :], in0=sg[si][:], in1=psu[:], op=ALU.mult),
                        reads=[sgb[si], psub], writes=[h1b[gi]], qname="dve")
                for blk in range(4):
                    yi = n_y % 2
                    n_y += 1
                    for cg in range(4):
                        ps, psb = next_ps(C)

                        def mm(e_, ps=ps, gi=gi, blk=blk, cg=cg, ws=ws):
                            ins = None
                            for fc in range(4):
                                ins = e_.matmul(ps[:], lhsT=h1[gi][:, fc, blk * 128:(blk + 1) * 128],
                                                rhs=wd[ws][:, fc, cg * 512:(cg + 1) * 512], start=(fc == 0), stop=(fc == 3))
                            return ins
                        P.op_cond(C.flag_i[0:1, e * 8 + g2 * 4 + blk:e * 8 + g2 * 4 + blk + 1], mm,
                                  reads=[h1b[gi], wb[ws]], writes=[psb])
                        if cg % 2 == 0:
                            P.op_cond(C.flag_i[0:1, e * 8 + g2 * 4 + blk:e * 8 + g2 * 4 + blk + 1], lambda e_, ps=ps, yi=yi, cg=cg: e_.copy(out=yev[yi][:, cg * 512:(cg + 1) * 512], in_=ps[:]),
                                      reads=[psb], writes=[yevb[yi]], qname="act")
                        else:
                            P.op_cond(C.flag_i[0:1, e * 8 + g2 * 4 + blk:e * 8 + g2 * 4 + blk + 1], lambda e_, ps=ps, yi=yi, cg=cg: e_.tensor_copy(out=yev[yi][:, cg * 512:(cg + 1) * 512], in_=ps[:]),
                                      reads=[psb], writes=[yevb[yi]], qname="dve")
                    r0 = slot0 + blk * 128
                    P.dma_cond(C.flag_i[0:1, e * 8 + g2 * 4 + blk:e * 8 + g2 * 4 + blk + 1], Y_d[r0:r0 + 128, :], yev[yi][:],
                               reads=[yevb[yi]])
        P.barrier()


def phase7_combine(C):
    nc, P = C.nc, C.P
    x1_d = C.dscr("x1", [TOWN, D], F32)
    Y_d = C.dscr("Y", [NSLOT, D], BF16)
    Ysh_d = C.dscr("Ysh", [TOWN, D], BF16)
    with ExitStack() as st:
        g2 = sb(nc, st, "p7_g2", [128, D], F32)
        b_g2 = Buf()
        P.dma("sp", g2[:], C.mod_d[0:1, 5 * D:6 * D].partition_broadcast(128), writes=[b_g2])
        yg = [sb(nc, st, f"p7_yg{i}", [128, D], BF16) for i in range(4)]
        ygb = [Buf() for _ in range(4)]
        for i in range(4):
            P.op("dve", lambda e, i=i: e.memset(yg[i][:], 0.0), writes=[ygb[i]])
        ysh = [sb(nc, st, f"p7_ysh{i}", [128, D], BF16) for i in range(2)]
        yshb = [Buf() for _ in range(2)]
        x1t = [sb(nc, st, f"p7_x1{i}", [128, D], F32) for i in range(2)]
        x1b = [Buf() for _ in range(2)]
        acc = [sb(nc, st, f"p7_acc{i}", [128, D], F32) for i in range(2)]
        accb = [Buf() for _ in range(2)]
        n_g = 0
        for i in range(16):
            s = i % 2
            P.dma("sp", ysh[s][:], Ysh_d[i * 128:(i + 1) * 128, :], writes=[yshb[s]])
            P.dma("sp", x1t[s][:], x1_d[i * 128:(i + 1) * 128, :], writes=[x1b[s]])
            for k in range(8):
                gi = n_g % 4
                n_g += 1
                P.dma_custom("pool", lambda e, gi=gi, k=k, i=i: e.indirect_dma_start(
                    out=yg[gi][:, :], out_offset=None, in_=Y_d[:, :],
                    in_offset=bass.IndirectOffsetOnAxis(ap=C.idx_all[:, i * 8 + k:i * 8 + k + 1], axis=0),
                    bounds_check=C.bc_reg, oob_is_err=False), reads=[C.b_idx[i]], writes=[ygb[gi]])
                src = ysh[s] if k == 0 else acc[s]
                srcb = yshb[s] if k == 0 else accb[s]
                P.op("dve", lambda e, gi=gi, k=k, i=i, src=src, s=s: e.scalar_tensor_tensor(
                    out=acc[s][:], in0=yg[gi][:], scalar=C.w_all[:, i * 8 + k:i * 8 + k + 1], in1=src[:], op0=ALU.mult, op1=ALU.add),
                    reads=[ygb[gi], C.b_idx[i], srcb], writes=[accb[s]])
            P.op("dve", lambda e, s=s: e.tensor_tensor(out=acc[s][:], in0=acc[s][:], in1=g2[:], op=ALU.mult),
                 reads=[b_g2], writes=[accb[s]])
            P.op("dve", lambda e, s=s: e.tensor_tensor(out=acc[s][:], in0=acc[s][:], in1=x1t[s][:], op=ALU.add),
                 reads=[x1b[s]], writes=[accb[s]])
            P.dma("sp", C.out[i * 128:(i + 1) * 128, :], acc[s][:], reads=[accb[s]], is_out=True)
        P.barrier()
```

```python
import os
import numpy as np
import concourse.bass as bass
import concourse.mybir as mybir
from concourse.bass_utils import run_bass_kernel_spmd

F32 = mybir.dt.float32
BF16 = mybir.dt.bfloat16
I32 = mybir.dt.int32
U32 = mybir.dt.uint32
AF = mybir.ActivationFunctionType
ALU = mybir.AluOpType
AX = mybir.AxisListType

D = 2048
NCH = 16
TOWN = 2048
TWIN = 4096
EPS = 1e-6
NEG = -30000.0


class Tok:
    __slots__ = ("sem", "val")

    def __init__(self, sem, val):
        self.sem = sem
        self.val = val


class Buf:
    __slots__ = ("name", "w", "r", "excl")

    def __init__(self, name="", excl=False):
        self.name = name
        self.w = None
        self.r = {}
        self.excl = excl


class DmaSem:
    __slots__ = ("sem", "total")

    def __init__(self, sem):
        self.sem = sem
        self.total = 0


class Queue:
    def __init__(self, name, eng, sem):
        self.name = name
        self.eng = eng
        self.sem = sem
        self.count = 0
        self.waited = {}
        self.ring = []
        self.ring_i = 0


class Prog:
    def __init__(self, nc, stack):
        self.nc = nc
        self.stack = stack
        self.q = {}
        for name, eng in (("pe", nc.tensor), ("act", nc.scalar), ("dve", nc.vector),
                          ("pool", nc.gpsimd), ("sp", nc.sync)):
            sem = stack.enter_context(nc.semaphore("cs_" + name))
            self.q[name] = Queue(name, eng, sem)
        for name, n in (("sp", 24), ("pool", 24), ("act", 8)):
            for i in range(n):
                sem = stack.enter_context(nc.semaphore(f"ds_{name}{i}"))
                self.q[name].ring.append(DmaSem(sem))
        self.out_toks = []
        self.reg_key = {}
        self.regs = {"pe": stack.enter_context(nc.tensor.register("pe_flag")),
                     "act": stack.enter_context(nc.scalar.register("act_flag")),
                     "dve": stack.enter_context(nc.vector.register("dve_flag"))}
        self.sp_reg = stack.enter_context(nc.sync.register("sp_flag"))

    def _wait(self, q, tok):
        key = id(tok.sem)
        if q.waited.get(key, 0) < tok.val:
            q.eng.wait_ge(tok.sem, tok.val)
            q.waited[key] = tok.val

    def _deps(self, q, reads, writes):
        for b in reads:
            if b.w is not None:
                self._wait(q, b.w)
        for b in writes:
            if b.w is not None:
                self._wait(q, b.w)
            for t in b.r.values():
                self._wait(q, t)

    def _record(self, tok, reads, writes):
        for b in reads:
            k = id(tok.sem)
            o = b.r.get(k)
            if o is None or o.val < tok.val:
                b.r[k] = tok
        for b in writes:
            b.w = tok
            b.r = {}

    def op(self, qname, fn, reads=(), writes=()):
        q = self.q[qname]
        ex = [b for b in reads if b.excl]
        if ex:
            reads = [b for b in reads if not b.excl]
            writes = list(writes) + ex
        self._deps(q, reads, writes)
        ins = fn(q.eng)
        q.count += 1
        ins.then_inc(q.sem, 1)
        if qname == "pe":
            q.waited[id(q.sem)] = q.count
        tok = Tok(q.sem, q.count)
        self._record(tok, reads, writes)
        return tok

    def op_cond(self, flag_ap, fn, reads=(), writes=(), qname="pe"):
        q = self.q[qname]
        eng = q.eng
        ex = [b for b in reads if b.excl]
        if ex:
            reads = [b for b in reads if not b.excl]
            writes = list(writes) + ex
        saved = dict(q.waited)
        reg = self.regs[qname]
        key = (flag_ap.offset, str(flag_ap.ap))
        if self.reg_key.get(qname) != key:
            eng.reg_load(reg, flag_ap)
            self.reg_key[qname] = key
        with eng.If_eq(reg, int(os.environ.get("P6_FLAGVAL", "1"))):
            self._deps(q, reads, writes)
            ins = fn(eng)
            ins.then_inc(q.sem, 1)
        with eng.Else():
            eng.drain()
            eng.sem_inc(q.sem, 1)
        q.count += 1
        q.waited = saved
        if qname == "pe":
            q.waited[id(q.sem)] = q.count
        tok = Tok(q.sem, q.count)
        self._record(tok, reads, writes)
        return tok

    def dma(self, qname, out, in_, reads=(), writes=(), is_out=False, **kw):
        q = self.q[qname]
        self._deps(q, reads, writes)
        ds = q.ring[q.ring_i]
        q.ring_i = (q.ring_i + 1) % len(q.ring)
        if ds.total > 0:
            self._wait(q, Tok(ds.sem, ds.total))
        q.eng.dma_start(out=out, in_=in_, **kw).then_inc(ds.sem, 16)
        ds.total += 16
        tok = Tok(ds.sem, ds.total)
        self._record(tok, reads, writes)
        if is_out:
            self.out_toks.append(tok)
        return tok

    def dma_cond(self, flag_ap, out, in_, reads=(), writes=()):
        q = self.q["sp"]
        eng = q.eng
        ds = q.ring[q.ring_i]
        q.ring_i = (q.ring_i + 1) % len(q.ring)
        if ds.total > 0:
            self._wait(q, Tok(ds.sem, ds.total))
        saved = dict(q.waited)
        key = (flag_ap.offset, str(flag_ap.ap))
        if self.reg_key.get("sp") != key:
            eng.reg_load(self.sp_reg, flag_ap)
            self.reg_key["sp"] = key
        with eng.If_eq(self.sp_reg, int(os.environ.get("P6_DMAFLAG", "1"))):
            self._deps(q, reads, writes)
            eng.dma_start(out=out, in_=in_).then_inc(ds.sem, 16)
        with eng.Else():
            eng.sem_inc(ds.sem, 16)
        q.waited = saved
        ds.total += 16
        tok = Tok(ds.sem, ds.total)
        self._record(tok, reads, writes)
        return tok

    def dma_custom(self, qname, fn, reads=(), writes=()):
        q = self.q[qname]
        self._deps(q, reads, writes)
        ds = q.ring[q.ring_i]
        q.ring_i = (q.ring_i + 1) % len(q.ring)
        if ds.total > 0:
            self._wait(q, Tok(ds.sem, ds.total))
        fn(q.eng).then_inc(ds.sem, 16)
        ds.total += 16
        tok = Tok(ds.sem, ds.total)
        self._record(tok, reads, writes)
        return tok

    def barrier(self):
        toks = []
        for q in self.q.values():
            if q.count > 0:
                toks.append(Tok(q.sem, q.count))
            for ds in q.ring:
                if ds.total > 0:
                    toks.append(Tok(ds.sem, ds.total))
        for q in self.q.values():
            for t in toks:
                if t.sem is not q.sem:
                    self._wait(q, t)

    def finish(self):
        q = self.q["sp"]
        for t in self.out_toks:
            self._wait(q, t)
        self.barrier()


from contextlib import ExitStack

W_HQ, W_HF, W_HI, W_HG, W_DQ, W_DK, W_DV, W_GA, W_GB = 0, 1024, 2048, 3072, 4096, 5120, 6144, 7168, 9216
HG_SCALE = 128 ** -0.5
DA_SCALE = 64 ** -0.5
LAMBDA_INIT = 0.8 - 0.6 * 1.0


class Ctx:
    pass


class StopBuild(Exception):
    pass


_SB_N = [0]


def sb(nc, st, name, shape, dt):
    _SB_N[0] += 1
    return st.enter_context(nc.sbuf_tensor(f"s{_SB_N[0]}_{name}", list(shape), dt))


def build(debug=None):
    nc = bass.Bass("TRN2", target_bir_lowering=False)
    C = Ctx()
    C.nc = nc
    C.debug = debug
    C.in_names = []
    C.dram = {}

    def din(name, shape, dt=F32):
        if name not in C.dram:
            C.dram[name] = nc.dram_tensor(name, list(shape), dt, kind="ExternalInput").ap()
            C.in_names.append(name)
        return C.dram[name]

    def dscr(name, shape, dt):
        if name not in C.dram:
            C.dram[name] = nc.dram_tensor(name, list(shape), dt, kind="Internal").ap()
        return C.dram[name]

    C.din, C.dscr = din, dscr
    C.out = nc.dram_tensor("out", [TOWN, D], F32, kind="ExternalOutput").ap()
    if debug is not None:
        C.dbg = nc.dram_tensor("dbg", list(debug[1]), debug[2], kind="ExternalOutput").ap()
    dscr("Xs", [64 * 1024, D], BF16)
    dscr("Y", [64 * 1024, D], BF16)
    C.mod_d = dscr("mod_d", [1, 6 * D], F32)

    def stop(tag, src=None):
        if debug is not None and debug[0] == tag:
            if src is not None:
                C.P.barrier()
                C.P.dma("sp", C.dbg, src, is_out=True)
            C.P.finish()
            return True
        return False

    with ExitStack() as st:
        P = Prog(nc, st)
        C.P = P
        C.ps = [st.enter_context(nc.psum_tensor(f"ps{i}", [128, 512], F32)) for i in range(6)]
        C.psb = [Buf(f"ps{i}", excl=True) for i in range(6)]
        C.pst = [st.enter_context(nc.psum_tensor(f"pst{i}", [128, 1024], BF16)) for i in range(2)]
        C.pstb = [Buf(f"pst{i}", excl=True) for i in range(2)]
        C.ps_i = 0
        C.pst_i = 0
        load_consts(C, st)
        phase0_mod(C)
        if stop("mod", C.mod_d):
            return nc, C
        C.S32 = sb(nc, st, "S32", [128, 8, 128], F32)
        C.Sbf = sb(nc, st, "Sbf", [128, 8, 128], BF16)
        C.S32b = [Buf() for _ in range(8)]
        C.Sbfb = [Buf() for _ in range(8)]
        P.op("dve", lambda e: e.memset(C.S32[:], 0.0), writes=C.S32b)
        P.op("dve", lambda e: e.memset(C.Sbf[:], 0.0), writes=C.Sbfb)
        half_pass(C, 0)
        half_pass(C, 1)
        if stop("hgrn", C.dscr("hgoT", [128, 8, TOWN], BF16)):
            return nc, C
        if stop("KT", C.dscr("KT", [8, 128, TWIN], BF16)):
            return nc, C
        if stop("V", C.dscr("V", [8, 128, 32, 129], BF16)):
            return nc, C
        if stop("SG", C.dscr("SG", [2, 16, 128, TOWN], BF16)):
            return nc, C
        C.stopped = False
        attention(C)
        if C.stopped:
            return nc, C
        if stop("attn", C.dscr("daoT", [128, 8, TOWN], BF16)):
            return nc, C
        phase4_mix_out(C)
        if C.stopped:
            return nc, C
        if stop("x1", C.dscr("x1", [TOWN, D], F32)):
            return nc, C
        phase5_route(C, st)
        if debug is not None and debug[0] == "route":
            P.barrier()
            P.dma("sp", C.dbg[:, :, 0:8], C.idx_all[:].bitcast(F32).rearrange("p (t k) -> p t k", k=8), is_out=True)
            P.dma("sp", C.dbg[:, :, 8:16], C.w_all[:].rearrange("p (t k) -> p t k", k=8), is_out=True)
            P.finish()
            return nc, C
        phase6_experts(C)
        phase7_combine(C)
        P.finish()
    return nc, C


def next_ps(C):
    i = C.ps_i
    C.ps_i = (i + 1) % len(C.ps)
    return C.ps[i], C.psb[i]


def next_pst(C):
    i = C.pst_i
    C.pst_i = (i + 1) % len(C.pst)
    return C.pst[i], C.pstb[i]


def load_consts(C, st):
    nc, P = C.nc, C.P
    C.ident_f = sb(nc, st, "ident_f", [128, 128], F32)
    C.ident = sb(nc, st, "ident", [128, 128], BF16)
    C.b_ident = Buf()
    P.dma("sp", C.ident_f[:], C.din("ident", [128, 128])[:, :], writes=[C.b_ident])
    P.op("dve", lambda e: e.tensor_copy(out=C.ident[:], in_=C.ident_f[:]), reads=[C.b_ident], writes=[C.b_ident])
    C.cst = sb(nc, st, "cst", [128, 2], F32)
    C.b_cst = Buf()
    P.op("dve", lambda e: e.memset(C.cst[:], EPS), writes=[C.b_cst])
    C.pm = sb(nc, st, "pm", [128, 2], F32)
    C.b_pm = Buf()
    P.dma("sp", C.pm[:], C.din("pm", [128, 2])[:, :], writes=[C.b_pm])


def phase0_mod(C):
    nc, P = C.nc, C.P
    c_col = C.din("c_col", [128, NCH])
    ada_w = C.din("ada_w", [D, 6 * D])
    ada_b = C.din("ada_b", [1, 6 * D])
    with ExitStack() as st:
        ccol = sb(nc, st, "p0_ccol", [128, NCH], F32)
        cact = sb(nc, st, "p0_cact", [128, NCH], BF16)
        wt = [sb(nc, st, f"p0_w{i}", [128, NCH, 512], BF16) for i in range(2)]
        wtb = [Buf() for _ in range(2)]
        bt = sb(nc, st, "p0_b", [1, 6 * D], F32)
        mt = sb(nc, st, "p0_m", [1, 6 * D], F32)
        b_ccol, b_cact, b_bt, b_mt = Buf(), Buf(), Buf(), Buf()
        P.dma("sp", ccol[:], c_col[:, :], writes=[b_ccol])
        P.dma("sp", bt[:], ada_b[:, :], writes=[b_bt])
        P.op("act", lambda e: e.activation(out=cact[:], in_=ccol[:], func=AF.Silu),
             reads=[b_ccol], writes=[b_cact])
        aw = ada_w.rearrange("(c p) n -> p c n", p=128)
        for g in range(24):
            s = g % 2
            P.dma("pool", wt[s][:], aw[:, :, g * 512:(g + 1) * 512], writes=[wtb[s]])
            ps, psb = next_ps(C)

            def mm(e, s=s, ps=ps):
                ins = None
                for j in range(NCH):
                    ins = e.matmul(ps[0:1, :], lhsT=cact[:, j:j + 1], rhs=wt[s][:, j, :],
                                   start=(j == 0), stop=(j == NCH - 1))
                return ins
            P.op("pe", mm, reads=[b_cact, wtb[s]], writes=[psb])
            P.op("dve", lambda e, g=g, ps=ps: e.tensor_tensor(
                out=mt[0:1, g * 512:(g + 1) * 512], in0=ps[0:1, :],
                in1=bt[0:1, g * 512:(g + 1) * 512], op=ALU.add),
                reads=[psb, b_bt], writes=[b_mt])
        P.dma("sp", C.mod_d[:, :], mt[:], reads=[b_mt])
        P.barrier()


def bcast_load(C, dst, src_row):
    return src_row.partition_broadcast(128)


def make_hT(C, st, half, hT, hTb):
    nc, P = C.nc, C.P
    xw = C.din("xw", [TWIN, D])
    norm1_g = C.din("norm1_g", [1, D])
    with ExitStack() as s2:
        G = sb(nc, s2, "n1_G", [128, D], F32)
        S = sb(nc, s2, "n1_S", [128, D], F32)
        t0 = sb(nc, s2, "n1_t0", [128, D], F32)
        bG, bS, bt0 = Buf(), Buf(), Buf()
        P.dma("sp", G[:], C.mod_d[0:1, D:2 * D].partition_broadcast(128), writes=[bG])
        P.dma("sp", t0[:], norm1_g[0:1, :].partition_broadcast(128), writes=[bt0])
        P.dma("sp", S[:], C.mod_d[0:1, 0:D].partition_broadcast(128), writes=[bS])
        P.op("dve", lambda e: e.scalar_tensor_tensor(out=G[:], in0=G[:], scalar=1.0, in1=t0[:],
                                                     op0=ALU.add, op1=ALU.mult),
             reads=[bt0], writes=[bG])
        norm_tiles(C, s2, lambda i: xw[half * TOWN + i * 128: half * TOWN + (i + 1) * 128, :],
                   G, bG, S, bS, hT, hTb, "n1")
        P.barrier()


def norm_tiles(C, s2, src_fn, G, bG, S, bS, hT, hTb, pfx, h32_cb=None, col_fn=None, post_cb=None):
    nc, P = C.nc, C.P
    xt = [sb(nc, s2, f"{pfx}_x{i}", [128, D], F32) for i in range(2)]
    xtb = [Buf() for _ in range(2)]
    junk = sb(nc, s2, f"{pfx}_junk", [128, D], BF16)
    bjunk = Buf()
    tmp = sb(nc, s2, f"{pfx}_tmp", [128, D], F32)
    btmp = Buf()
    hb = [sb(nc, s2, f"{pfx}_hb{i}", [128, D], BF16) for i in range(2)]
    hbb = [Buf() for _ in range(2)]
    ss = sb(nc, s2, f"{pfx}_ss", [128, 4], F32)
    bss = Buf()
    if col_fn is None:
        col_fn = lambda i: i * 128
    for i in range(16):
        s = i % 2
        c0 = col_fn(i)
        P.dma("sp", xt[s][:], src_fn(i), writes=[xtb[s]])
        P.op("act", lambda e, s=s: e.activation(out=junk[:], in_=xt[s][:], func=AF.Square,
                                                accum_out=ss[:, 0:1]),
             reads=[xtb[s]], writes=[bjunk, bss])
        P.op("act", lambda e: e.activation(out=ss[:, 1:2], in_=ss[:, 0:1], func=AF.Ln,
                                           scale=1.0 / D, bias=C.cst[:, 0:1]),
             reads=[bss, C.b_cst], writes=[bss])
        P.op("act", lambda e: e.activation(out=ss[:, 2:3], in_=ss[:, 1:2], func=AF.Exp, scale=-0.5),
             reads=[bss], writes=[bss])
        P.op("dve", lambda e, s=s: e.scalar_tensor_tensor(out=tmp[:], in0=xt[s][:], scalar=ss[:, 2:3],
                                                          in1=G[:], op0=ALU.mult, op1=ALU.mult),
             reads=[xtb[s], bss, bG], writes=[btmp])
        if h32_cb is not None:
            h32_cb(i, tmp, btmp, S, bS, hb[s], hbb[s])
        else:
            P.op("dve", lambda e, s=s: e.tensor_tensor(out=hb[s][:], in0=tmp[:], in1=S[:], op=ALU.add),
                 reads=[btmp, bS], writes=[hbb[s]])
        for hlf in range(2):
            pt, ptb = next_pst(C)

            def tr(e, s=s, hlf=hlf, pt=pt):
                ins = None
                for c in range(8):
                    cc = hlf * 8 + c
                    ins = e.transpose(out=pt[:, c * 128:(c + 1) * 128], in_=hb[s][:, cc * 128:(cc + 1) * 128],
                                      identity=C.ident[:])
                return ins
            P.op("pe", tr, reads=[hbb[s], C.b_ident], writes=[ptb])
            eng = "act" if hlf == 0 else "dve"
            if eng == "act":
                P.op("act", lambda e, hlf=hlf, pt=pt, c0=c0: e.copy(
                    out=hT[:, hlf * 8:(hlf + 1) * 8, c0:c0 + 128],
                    in_=pt[:].rearrange("p (c t) -> p c t", t=128)),
                    reads=[ptb], writes=[hTb[i]])
            else:
                P.op("dve", lambda e, hlf=hlf, pt=pt, c0=c0: e.tensor_copy(
                    out=hT[:, hlf * 8:(hlf + 1) * 8, c0:c0 + 128],
                    in_=pt[:].rearrange("p (c t) -> p c t", t=128)),
                    reads=[ptb], writes=[hTb[i]])
        if post_cb is not None:
            post_cb(i, hb[s], hbb[s], c0)


def fm_proj(C, w, wb, c0, hT, hTb, tg, ncol=128):
    P = C.P
    ps, psb = next_ps(C)

    def mm(e):
        ins = None
        for j in range(NCH):
            ins = e.matmul(ps[0:ncol, :], lhsT=w[:, j, c0:c0 + ncol], rhs=hT[:, j, tg * 512:(tg + 1) * 512],
                           start=(j == 0), stop=(j == NCH - 1))
        return ins
    P.op("pe", mm, reads=[wb] + hTb[tg * 4:(tg + 1) * 4], writes=[psb])
    return ps, psb


def load_wcols(C, slot, slotb, col0, ncol):
    w_in = C.din("w_in", [D, 11264])
    wv = w_in.rearrange("(c p) n -> p c n", p=128)
    C.P.dma("pool", slot[:, :, 0:ncol], wv[:, :, col0:col0 + ncol], writes=[slotb])


def half_pass(C, half):
    nc, P = C.nc, C.P
    with ExitStack() as st:
        hT = sb(nc, st, "hT", [128, NCH, TOWN], BF16)
        hTb = [Buf() for _ in range(16)]
        make_hT(C, st, half, hT, hTb)
        if C.debug is not None and C.debug[0] == "hT" and half == 1:
            P.barrier()
            P.dma("sp", C.dbg, hT[:], is_out=True)
            return
        with ExitStack() as s2:
            hgrn_half(C, s2, half, hT, hTb)
            P.barrier()
        with ExitStack() as s2:
            attn_proj_half(C, s2, half, hT, hTb)
            P.barrier()
        if half == 1:
            with ExitStack() as s2:
                gates_proj(C, s2, hT, hTb)
                P.barrier()
        P.barrier()


def hgrn_half(C, st, half, hT, hTb):
    nc, P = C.nc, C.P
    own = half == 1
    T = TOWN
    NC64 = T // 64
    lbl = sb(nc, st, "hg_lbl", [128, 2, 8], F32)
    lb = sb(nc, st, "hg_lb", [128, 8], F32)
    oml = sb(nc, st, "hg_oml", [128, 8], F32)
    cmask = sb(nc, st, "hg_cmask", [128, T], F32)
    mask64 = sb(nc, st, "hg_mask64", [64, 64], F32)
    hgg = sb(nc, st, "hg_g", [64, 128], F32)
    b_c = Buf()
    P.dma("sp", lbl[:], C.din("lb_l", [128, 2, 8])[:, :, :], writes=[b_c])
    P.dma("sp", cmask[:], C.din("cmask", [128, T])[:, :], writes=[b_c])
    P.dma("sp", mask64[:], C.din("mask64", [64, 64])[:, :], writes=[b_c])
    P.dma("sp", hgg[:], C.din("hg_norm_g", [1, 128])[0:1, :].partition_broadcast(64), writes=[b_c])
    P.op("dve", lambda e: e.tensor_tensor(out=oml[:], in0=lbl[:, 0, :], in1=lbl[:, 1, :], op=ALU.subtract),
         reads=[b_c], writes=[b_c])
    P.op("act", lambda e: e.activation(out=lb[:], in_=oml[:], func=AF.Sigmoid), reads=[b_c], writes=[b_c])
    P.op("dve", lambda e: e.tensor_scalar(out=oml[:], in0=lb[:], scalar1=-1.0, scalar2=1.0,
                                          op0=ALU.mult, op1=ALU.add), reads=[b_c], writes=[b_c])
    def f32arr(n):
        return sb(nc, st, "hg_" + n, [128, T], F32), Buf()
    A, bA = f32arr("A")
    G, bG = f32arr("G")
    K, bK = f32arr("K")
    B, bB = f32arr("B")
    Dd, bD = A, bA
    E, bE = f32arr("E")
    if own:
        Q, bQ = f32arr("Q")
    sg = [sb(nc, st, f"hg_sg{i}", [128, 512], F32) for i in range(2)]
    sgb = [Buf() for _ in range(2)]
    fmT = [sg[i][:].bitcast(BF16)[:, 0:512] for i in range(2)]
    fmTb = sgb
    khatT = sb(nc, st, "hg_khatT", [128, T], BF16); b_khatT = Buf()
    khat = sb(nc, st, "hg_khat", [64, NC64, 128], BF16); b_khat = Buf()
    vh = sb(nc, st, "hg_vh", [64, NC64, 128], BF16); b_vh = Buf()
    dec = sb(nc, st, "hg_dec", [128, NC64], F32); b_dec = Buf()
    if own:
        qhat = sb(nc, st, "hg_qhat", [128, T], BF16); b_qhat = Buf()
        qtil = sb(nc, st, "hg_qtil", [128, T], BF16); b_qtil = Buf()
        ktil = sb(nc, st, "hg_ktil", [128, T], BF16); b_ktil = Buf()
        gate = sb(nc, st, "hg_gate", [64, NC64, 128], BF16); b_gate = Buf()
        STs = [sb(nc, st, f"hg_ST{i}", [64, 64], BF16) for i in range(2)]
        STb = [Buf() for _ in range(2)]
        hgoT, b_hgoT = khatT, b_khatT
        og = [sb(nc, st, f"hg_og{i}", [64, 128], F32) for i in range(2)]
        ogb = [Buf() for _ in range(2)]
        hgo = [sb(nc, st, f"hg_hgo{i}", [64, 128], BF16) for i in range(2)]
        hgob = [Buf() for _ in range(2)]
        oss = sb(nc, st, "hg_oss", [64, NC64], F32); b_oss = Buf()
        o_raw_p = [A[0:64, :].rearrange("p (n c) -> p n c", c=128), K[0:64, :].rearrange("p (n c) -> p n c", c=128)]
        b_oraw_p = [bA, bK]
        o_sq = E[0:64, :].rearrange("p (n c) -> p n c", c=128); b_osq = bE
        hgo_all = G[0:64, :].bitcast(BF16).rearrange("p (n c) -> p n c", c=128); b_hgoall = bG
        ojunk = sb(nc, st, "hg_ojunk", [64, 128], BF16); b_ojunk = Buf()
        hgoT_d = C.dscr("hgoT", [128, 8, TOWN], BF16)
    nw = 4 if own else 2
    wsl = [[sb(nc, st, f"hg_w{k}_{i}", [128, NCH, 128], BF16) for i in range(2)] for k in range(nw)]
    wslb = [[Buf() for i in range(2)] for k in range(nw)]
    fam_cols = [W_HF, W_HI, W_HQ, W_HG]

    def b3(ap):
        return ap.rearrange("p (n c) -> p n c", c=64)

    for h in range(8):
        s = h % 2
        for k in range(nw):
            load_wcols(C, wsl[k][s], wslb[k][s], fam_cols[k] + h * 128, 128)
        w_hf, w_hi = wsl[0][s], wsl[1][s]
        for tg in range(4):
            sl = slice(tg * 512, (tg + 1) * 512)
            ps, psb = fm_proj(C, w_hf, wslb[0][s], 0, hT, hTb, tg)
            P.op("act", lambda e, ps=ps, tg=tg: e.activation(out=sg[tg % 2][:], in_=ps[:], func=AF.Sigmoid),
                 reads=[psb], writes=[sgb[tg % 2]])
            P.op("dve", lambda e, tg=tg, sl=sl, h=h: e.tensor_scalar(
                out=A[:, sl], in0=sg[tg % 2][:], scalar1=oml[:, h:h + 1], scalar2=lb[:, h:h + 1],
                op0=ALU.mult, op1=ALU.add), reads=[sgb[tg % 2], b_c], writes=[bA])
            P.op("act", lambda e, sl=sl: e.activation(out=G[:, sl], in_=A[:, sl], func=AF.Ln),
                 reads=[bA], writes=[bG])
            P.op("dve", lambda e, sl=sl: e.tensor_scalar(out=K[:, sl], in0=A[:, sl], scalar1=-1.0, scalar2=1.0,
                                                         op0=ALU.mult, op1=ALU.add), reads=[bA], writes=[bK])
            if own:
                ps, psb = fm_proj(C, wsl[2][s], wslb[2][s], 0, hT, hTb, tg)
                P.op("act", lambda e, ps=ps, sl=sl: e.activation(out=Q[:, sl], in_=ps[:], func=AF.Silu),
                     reads=[psb], writes=[bQ])
        P.op("dve", lambda e: e.tensor_tensor_scan(out=B[:], data0=cmask[:], data1=G[:], initial=0.0,
                                                   op0=ALU.mult, op1=ALU.add), reads=[b_c, bG], writes=[bB])
        P.op("dve", lambda e: e.tensor_tensor(out=b3(Dd[:]), in0=b3(B[:])[:, :, 63:64].to_broadcast([128, NC64, 64]),
                                              in1=b3(B[:]), op=ALU.subtract), reads=[bB], writes=[bD])
        P.op("act", lambda e: e.activation(out=E[:], in_=Dd[:], func=AF.Exp), reads=[bD], writes=[bE])
        pmc = 1 if own else 0
        P.op("dve", lambda e, pmc=pmc: e.scalar_tensor_tensor(out=khatT[:], in0=K[:], scalar=C.pm[:, pmc:pmc + 1],
                                                              in1=E[:], op0=ALU.mult, op1=ALU.mult),
             reads=[bK, bE, C.b_pm], writes=[b_khatT])
        P.op("act", lambda e: e.activation(out=dec[:], in_=b3(B[:])[:, :, 63], func=AF.Exp), reads=[bB], writes=[b_dec])
        for c8 in range(NC64 // 8):
            pt, ptb = next_pst(C)

            def tr(e, c8=c8, pt=pt):
                ins = None
                for c in range(8):
                    cc = c8 * 8 + c
                    ins = e.transpose(out=pt[0:64, c * 128:(c + 1) * 128], in_=khatT[:, cc * 64:(cc + 1) * 64],
                                      identity=C.ident[:])
                return ins
            P.op("pe", tr, reads=[b_khatT, C.b_ident], writes=[ptb])
            P.op("act", lambda e, c8=c8, pt=pt: e.copy(out=khat[:, c8 * 8:(c8 + 1) * 8, :],
                                                       in_=pt[0:64, :].rearrange("p (c k) -> p c k", k=128)),
                 reads=[ptb], writes=[b_khat])
        if own:
            P.op("act", lambda e: e.activation(out=E[:], in_=B[:], func=AF.Exp), reads=[bB], writes=[bE])
            P.op("dve", lambda e: e.scalar_tensor_tensor(out=qhat[:], in0=Q[:], scalar=HG_SCALE, in1=E[:],
                                                         op0=ALU.mult, op1=ALU.mult), reads=[bQ, bE], writes=[b_qhat])
            P.op("dve", lambda e: e.tensor_tensor(out=b3(Dd[:]), in0=b3(B[:]),
                                                  in1=b3(B[:])[:, :, 31:32].to_broadcast([128, NC64, 64]),
                                                  op=ALU.subtract), reads=[bB], writes=[bD])
            P.op("act", lambda e: e.activation(out=E[:], in_=Dd[:], func=AF.Exp), reads=[bD], writes=[bE])
            P.op("dve", lambda e: e.scalar_tensor_tensor(out=qtil[:], in0=Q[:], scalar=HG_SCALE, in1=E[:],
                                                         op0=ALU.mult, op1=ALU.mult), reads=[bQ, bE], writes=[b_qtil])
            P.op("act", lambda e: e.activation(out=E[:], in_=Dd[:], func=AF.Exp, scale=-1.0), reads=[bD], writes=[bE])
            P.op("dve", lambda e: e.tensor_tensor(out=ktil[:], in0=K[:], in1=E[:], op=ALU.mult),
                 reads=[bK, bE], writes=[b_ktil])
        fams = [(w_hi, wslb[1][s], vh, b_vh, AF.Copy)]
        if own:
            fams.append((wsl[3][s], wslb[3][s], gate, b_gate, AF.Silu))
        for (w, wb, dst, dstb, fn) in fams:
            for tg in range(4):
                ps, psb = fm_proj(C, w, wb, 0, hT, hTb, tg)
                fi = tg % 2
                P.op("act", lambda e, ps=ps, fi=fi, fn=fn: e.activation(out=fmT[fi], in_=ps[:], func=fn),
                     reads=[psb], writes=[fmTb[fi]])
                pt, ptb = next_pst(C)

                def trv(e, pt=pt, fi=fi):
                    ins = None
                    for c in range(8):
                        ins = e.transpose(out=pt[0:64, c * 128:(c + 1) * 128], in_=fmT[fi][:, c * 64:(c + 1) * 64],
                                          identity=C.ident[:])
                    return ins
                P.op("pe", trv, reads=[fmTb[fi], C.b_ident], writes=[ptb])
                P.op("dve", lambda e, pt=pt, tg=tg, dst=dst: e.tensor_copy(
                    out=dst[:, tg * 8:(tg + 1) * 8, :], in_=pt[0:64, :].rearrange("p (c k) -> p c k", k=128)),
                    reads=[ptb], writes=[dstb])
        for c in range(NC64):
            cs = slice(c * 64, (c + 1) * 64)
            if own:
                ps_st, psb_st = next_ps(C)
                P.op("pe", lambda e, ps_st=ps_st, cs=cs: e.matmul(ps_st[0:64, 0:64], lhsT=ktil[:, cs], rhs=qtil[:, cs],
                                                                start=True, stop=True),
                     reads=[b_ktil, b_qtil], writes=[psb_st])
                si = c % 2
                P.op("dve", lambda e, ps_st=ps_st, si=si: e.tensor_tensor(out=STs[si][:], in0=ps_st[0:64, 0:64],
                                                                         in1=mask64[:], op=ALU.mult),
                     reads=[psb_st, b_c], writes=[STb[si]])
                ps_o, psb_o = next_ps(C)

                def mmo(e, ps_o=ps_o, si=si, c=c, cs=cs, h=h):
                    e.matmul(ps_o[0:64, 0:128], lhsT=STs[si][:], rhs=vh[:, c, :], start=True, stop=False)
                    return e.matmul(ps_o[0:64, 0:128], lhsT=qhat[:, cs], rhs=C.Sbf[:, h, :], start=False, stop=True)
                P.op("pe", mmo, reads=[STb[si], b_vh, b_qhat, C.Sbfb[h]], writes=[psb_o])
            ps_kv, psb_kv = next_ps(C)
            P.op("pe", lambda e, ps_kv=ps_kv, c=c: e.matmul(ps_kv[:, 0:128], lhsT=khat[:, c, :], rhs=vh[:, c, :],
                                                           start=True, stop=True),
                 reads=[b_khat, b_vh], writes=[psb_kv])
            P.op("dve", lambda e, ps_kv=ps_kv, c=c, h=h: e.scalar_tensor_tensor(
                out=C.S32[:, h, :], in0=C.S32[:, h, :], scalar=dec[:, c:c + 1], in1=ps_kv[:, 0:128],
                op0=ALU.mult, op1=ALU.add), reads=[psb_kv, b_dec], writes=[C.S32b[h]])
            if own or c == NC64 - 1:
                P.op("dve", lambda e, h=h: e.tensor_copy(out=C.Sbf[:, h, :], in_=C.S32[:, h, :]),
                     reads=[C.S32b[h]], writes=[C.Sbfb[h]])
            if own:
                P.op("act", lambda e, ps_o=ps_o, c=c: e.copy(out=o_raw_p[c // 16][:, c % 16, :], in_=ps_o[0:64, 0:128]),
                     reads=[psb_o], writes=[b_oraw_p[c // 16]])
        if own:
            HN = NC64 // 2
            for hf_ in range(2):
                cs_ = slice(hf_ * HN, (hf_ + 1) * HN)
                P.op("dve", lambda e, hf_=hf_: e.tensor_tensor(out=o_sq, in0=o_raw_p[hf_], in1=o_raw_p[hf_], op=ALU.mult),
                     reads=[b_oraw_p[hf_]], writes=[b_osq])
                P.op("dve", lambda e, cs_=cs_: e.tensor_reduce(out=oss[:, cs_], in_=o_sq, axis=AX.X, op=ALU.add), reads=[b_osq], writes=[b_oss])
            P.op("act", lambda e: e.activation(out=oss[:, 0:NC64], in_=oss[:, 0:NC64], func=AF.Ln, scale=1.0 / 128,
                                               bias=C.cst[0:64, 0:1]), reads=[C.b_cst], writes=[b_oss])
            P.op("act", lambda e: e.activation(out=oss[:, 0:NC64], in_=oss[:, 0:NC64], func=AF.Exp, scale=-0.5), reads=[], writes=[b_oss])
            for hf_ in range(2):
                cs_ = slice(hf_ * HN, (hf_ + 1) * HN)
                P.op("dve", lambda e, cs_=cs_, hf_=hf_: e.tensor_tensor(out=o_sq, in0=o_raw_p[hf_],
                                                                        in1=oss[:, cs_].unsqueeze(2).to_broadcast([64, HN, 128]), op=ALU.mult),
                     reads=[b_oraw_p[hf_], b_oss], writes=[b_osq])
                P.op("dve", lambda e: e.tensor_tensor(out=o_sq, in0=o_sq, in1=hgg[:].unsqueeze(1).to_broadcast([64, HN, 128]), op=ALU.mult),
                     reads=[b_c], writes=[b_osq])
                P.op("dve", lambda e, cs_=cs_: e.tensor_tensor(out=hgo_all[:, cs_, :], in0=o_sq, in1=gate[:, cs_, :], op=ALU.mult),
                     reads=[b_osq, b_gate], writes=[b_hgoall])
            for c8 in range(NC64 // 8):
                pt_o, ptb_o = next_pst(C)

                def tro(e, c8=c8, pt_o=pt_o):
                    ins = None
                    for c in range(8):
                        ins = e.transpose(out=pt_o[:, c * 64:(c + 1) * 64], in_=hgo_all[:, c8 * 8 + c, :], identity=C.ident[0:64, 0:64])
                    return ins
                P.op("pe", tro, reads=[b_hgoall, C.b_ident], writes=[ptb_o])
                P.op("act", lambda e, pt_o=pt_o, c8=c8: e.copy(out=hgoT[:, c8 * 512:(c8 + 1) * 512], in_=pt_o[:, 0:512]),
                     reads=[ptb_o], writes=[b_hgoT])
        if own:
            P.dma("sp", hgoT_d[:, h, :], hgoT[:], reads=[b_hgoT], writes=[])


def host_array(name, inputs, core):
    b, half = core // 2, core % 2
    f = np.float32
    if name == "xw":
        x = inputs["x"]
        xw = np.zeros((TWIN, D), f)
        if half == 1:
            xw[:] = x[b]
        else:
            xw[TOWN:] = x[b, :TOWN]
        return xw
    if name == "c_col":
        return np.ascontiguousarray(np.asarray(inputs["c"][b], f).reshape(NCH, 128).T)
    if name == "ada_b":
        return np.ascontiguousarray(inputs["ada_b"][0][None, :])
    if name in ("ada_w", "w_in", "w_branch_a", "w_branch_b", "w_out", "w_sh_gate", "w_sh_up", "w_sh_down",
                "w_exp_gate", "w_exp_up", "w_exp_down", "router_w"):
        return np.ascontiguousarray(inputs[name][0])
    if name in ("norm1_g", "norm2_g", "hg_norm_g", "da_norm_g", "router_bias"):
        return np.ascontiguousarray(np.asarray(inputs[name][0], f)[None, :])
    if name == "ident":
        return np.eye(128, dtype=f)
    if name == "pm":
        pm = np.ones((128, 2), f)
        pm[:, 0] = float(half)
        return pm
    if name == "lb_l":
        l = np.asarray(inputs["lb_logits"], f)
        return np.ascontiguousarray(l.reshape(2, 8, 128).transpose(2, 0, 1))
    if name == "cmask":
        m = np.ones((128, TOWN), f)
        m[:, ::64] = 0.0
        return m
    if name == "blockones":
        m = np.zeros((128, 128), f)
        m[:64, :64] = 1.0
        m[64:, 64:] = 1.0
        return m
    if name == "qk_g":
        g = np.zeros((128, 2), f)
        g[:, 0] = np.tile(np.asarray(inputs["k_norm_g"][0], f), 2)
        g[:, 1] = np.tile(np.asarray(inputs["q_norm_g"][0], f), 2)
        return g
    if name == "abias":
        tab = np.asarray(inputs["rel_bias"], f)
        r = np.arange(128)[:, None]
        u = np.arange(1024)[None, :]
        dist = u - r - 384
        n = np.maximum(dist, 1).astype(np.float32)
        large = 16 + (np.log(n / 16) / np.log(128 / 16) * 16).astype(np.int32)
        large = np.minimum(large, 31)
        bucket = np.where(dist < 16, np.maximum(dist, 0), large)
        out = np.empty((8, 128, 1024), f)
        for hh in range(8):
            out[hh] = np.where(dist >= 0, tab[bucket, hh], f(NEG))
        return out
    if name == "farb":
        tab = np.asarray(inputs["rel_bias"], f)
        o = np.empty((128, 17), f)
        for hh in range(8):
            o[:, 2 * hh] = tab[31, hh] if half == 1 else f(NEG)
            o[:, 2 * hh + 1] = tab[31, hh]
        o[:, 16] = 0.0 if half == 1 else f(NEG)
        return o
    if name == "lam4":
        return np.stack([np.asarray(inputs[k][0], f) for k in ("lambda_q1", "lambda_k1", "lambda_q2", "lambda_k2")])
    if name == "thr8":
        return np.tile(np.arange(8, dtype=f) * 128.0, 64)[None, :]
    if name == "ecap":
        return (np.arange(64, dtype=f) * CAP)[None, :]
    if name == "ustrict":
        t = np.arange(128)
        return (t[:, None] < t[None, :]).astype(f)
    if name == "mask64":
        s = np.arange(64)
        return (s[:, None] <= s[None, :]).astype(f)
    raise KeyError(name)


def make_in_maps(C, inputs, cores):
    inputs = {k: np.asarray(v) for k, v in inputs.items()}
    return [{n: host_array(n, inputs, core) for n in C.in_names} for core in cores]


def kernel(**inputs):
    nc, C = build()
    maps = make_in_maps(C, inputs, list(range(8)))
    res = run_bass_kernel_spmd(nc, maps, core_ids=list(range(8)))
    out = np.zeros((4, 4096, D), np.float32)
    for core in range(8):
        b, half = core // 2, core % 2
        out[b, half * TOWN:(half + 1) * TOWN] = np.asarray(res.results[core]["out"])
    return out


def attn_proj_half(C, st, half, hT, hTb):
    nc, P = C.nc, C.P
    own = half == 1
    KT_d = C.dscr("KT", [8, 128, TWIN], BF16)
    QT_d = C.dscr("QT", [8, 128, TOWN], BF16)
    V_d = C.dscr("V", [8, 128, 32, 129], BF16)
    bones = sb(nc, st, "ap_bones", [128, 128], BF16)
    bones_f = sb(nc, st, "ap_bones_f", [128, 128], F32)
    gcol = sb(nc, st, "ap_gcol", [128, 2], F32)
    b_c = Buf()
    P.dma("sp", bones_f[:], C.din("blockones", [128, 128])[:, :], writes=[b_c])
    P.op("dve", lambda e: e.tensor_copy(out=bones[:], in_=bones_f[:]), reads=[b_c], writes=[b_c])
    P.dma("sp", gcol[:], C.din("qk_g", [128, 2])[:, :], writes=[b_c])
    P.op("dve", lambda e: e.tensor_scalar(out=gcol[:, 1:2], in0=gcol[:, 1:2], scalar1=DA_SCALE, scalar2=None,
                                          op0=ALU.mult), reads=[b_c], writes=[b_c])
    nw = 3 if own else 2
    wsl = [[sb(nc, st, f"ap_w{k}_{i}", [128, NCH, 128], BF16) for i in range(2)] for k in range(nw)]
    wslb = [[Buf() for i in range(2)] for k in range(nw)]
    fam_cols = [W_DK, W_DV, W_DQ]
    sq = [sb(nc, st, f"ap_sq{i}", [128, 512], BF16) for i in range(2)]
    sqb = [Buf() for _ in range(2)]
    raw = [sb(nc, st, f"ap_raw{i}", [128, 512], F32) for i in range(2)]
    rawb = [Buf() for _ in range(2)]
    rr = [sb(nc, st, f"ap_rr{i}", [128, 512], F32) for i in range(2)]
    rrb = [Buf() for _ in range(2)]
    xn = [sb(nc, st, f"ap_xn{i}", [128, TOWN], BF16) for i in range(2)]
    xnb = [Buf() for _ in range(2)]
    fmT = [sb(nc, st, f"ap_fmT{i}", [128, 512], BF16) for i in range(2)]
    fmTb = [Buf() for _ in range(2)]
    Vs = [sb(nc, st, f"ap_V{i}", [128, 16, 129], BF16) for i in range(2)]
    Vsb = [Buf() for _ in range(2)]
    for i in range(2):
        P.op("dve", lambda e, i=i: e.memset(Vs[i][:, :, 128:129], 1.0), writes=[Vsb[i]])
    it = 0
    for h in range(8):
        s = h % 2
        for k in range(nw):
            load_wcols(C, wsl[k][s], wslb[k][s], fam_cols[k] + h * 128, 128)
        for (k, gc, dst) in ([(0, 0, KT_d[h, :, half * TOWN:(half + 1) * TOWN])] + ([(2, 1, QT_d[h, :, :])] if own else [])):
            xi = it % 2
            it += 1
            for tg in range(4):
                sl = slice(tg * 512, (tg + 1) * 512)
                ps, psb = fm_proj(C, wsl[k][s], wslb[k][s], 0, hT, hTb, tg)
                ti = tg % 2
                P.op("act", lambda e, ps=ps, ti=ti: e.activation(out=sq[ti][:], in_=ps[:], func=AF.Square),
                     reads=[psb], writes=[sqb[ti]])
                P.op("dve", lambda e, ps=ps, ti=ti: e.tensor_copy(out=raw[ti][:], in_=ps[:]),
                     reads=[psb], writes=[rawb[ti]])
                ps2, psb2 = next_ps(C)
                P.op("pe", lambda e, ps2=ps2, ti=ti: e.matmul(ps2[:], lhsT=bones[:], rhs=sq[ti][:], start=True, stop=True),
                     reads=[sqb[ti], b_c], writes=[psb2])
                P.op("act", lambda e, ps2=ps2, ti=ti: e.activation(out=rr[ti][:], in_=ps2[:], func=AF.Ln, scale=1.0 / 64,
                                                                  bias=C.cst[:, 0:1]), reads=[psb2, C.b_cst], writes=[rrb[ti]])
                P.op("act", lambda e, ti=ti: e.activation(out=rr[ti][:], in_=rr[ti][:], func=AF.Exp, scale=-0.5),
                     reads=[], writes=[rrb[ti]])
                P.op("dve", lambda e, ti=ti, sl=sl, xi=xi, gc=gc: e.scalar_tensor_tensor(
                    out=xn[xi][:, sl], in0=raw[ti][:], scalar=gcol[:, gc:gc + 1], in1=rr[ti][:],
                    op0=ALU.mult, op1=ALU.mult), reads=[rawb[ti], rrb[ti], b_c], writes=[xnb[xi]])
            P.dma("sp", dst, xn[xi][:], reads=[xnb[xi]])
        vi = h % 2
        for tg in range(4):
            ps, psb = fm_proj(C, wsl[1][s], wslb[1][s], 0, hT, hTb, tg)
            fi = tg % 2
            P.op("act", lambda e, ps=ps, fi=fi: e.copy(out=fmT[fi][:], in_=ps[:]), reads=[psb], writes=[fmTb[fi]])
            pt, ptb = next_pst(C)

            def trv(e, pt=pt, fi=fi):
                ins = None
                for t in range(4):
                    ins = e.transpose(out=pt[:, t * 128:(t + 1) * 128], in_=fmT[fi][:, t * 128:(t + 1) * 128], identity=C.ident[:])
                return ins
            P.op("pe", trv, reads=[fmTb[fi], C.b_ident], writes=[ptb])
            P.op("dve", lambda e, pt=pt, tg=tg, vi=vi: e.tensor_copy(
                out=Vs[vi][:, tg * 4:(tg + 1) * 4, 0:128], in_=pt[:, 0:512].rearrange("p (t k) -> p t k", k=128)),
                reads=[ptb], writes=[Vsb[vi]])
        P.dma("sp", V_d[h, :, half * 16:(half + 1) * 16, :], Vs[vi][:], reads=[Vsb[vi]])


def gates_proj(C, st, hT, hTb):
    nc, P = C.nc, C.P
    SG_d = C.dscr("SG", [2, 16, 128, TOWN], BF16)
    wsl = [sb(nc, st, f"gp_w{i}", [128, NCH, 512], BF16) for i in range(2)]
    wslb = [Buf() for _ in range(2)]
    sg = [sb(nc, st, f"gp_sg{i}", [128, TOWN], BF16) for i in range(2)]
    sgb = [Buf() for _ in range(2)]
    n = 0
    for fam in range(2):
        for cg in range(4):
            s = (fam * 4 + cg) % 2
            load_wcols(C, wsl[s], wslb[s], (W_GA if fam == 0 else W_GB) + cg * 512, 512)
            for cc in range(4):
                si = n % 2
                n += 1
                for tg in range(4):
                    ps, psb = fm_proj(C, wsl[s], wslb[s], cc * 128, hT, hTb, tg)
                    P.op("act", lambda e, ps=ps, si=si, tg=tg: e.activation(
                        out=sg[si][:, tg * 512:(tg + 1) * 512], in_=ps[:], func=AF.Sigmoid),
                        reads=[psb], writes=[sgb[si]])
                P.dma("sp", SG_d[fam, cg * 4 + cc, :, :], sg[si][:], reads=[sgb[si]])


def attention(C):
    nc, P = C.nc, C.P
    KT_d = C.dscr("KT", [8, 128, TWIN], BF16)
    QT_d = C.dscr("QT", [8, 128, TOWN], BF16)
    V_d = C.dscr("V", [8, 128, 32, 129], BF16)
    daoT_d = C.dscr("daoT", [128, 8, TOWN], BF16)
    abias = C.din("abias", [8, 128, 1024])
    farb = C.din("farb", [128, 17])
    lam4 = C.din("lam4", [4, 64])
    with ExitStack() as st:
        KT = [sb(nc, st, f"at_KT{i}", [128, TWIN], BF16) for i in range(2)]
        QT = [sb(nc, st, f"at_QT{i}", [128, TOWN], BF16) for i in range(2)]
        V = [sb(nc, st, f"at_V{i}", [128, 32, 129], BF16) for i in range(2)]
        Wb = [sb(nc, st, f"at_Wb{i}", [128, 1024], F32) for i in range(2)]
        hb = [Buf() for _ in range(2)]
        fb = sb(nc, st, "at_farb", [128, 17], F32)
        l4 = sb(nc, st, "at_l4", [128, 4, 64], F32)
        lt = sb(nc, st, "at_lt", [128, 2, 64], F32)
        lam = sb(nc, st, "at_lam", [128, 4], F32)
        dag = sb(nc, st, "at_dag", [128, 128], F32)
        b_c = Buf()
        P.dma("sp", fb[:], farb[:, :], writes=[b_c])
        for i in range(4):
            P.dma("sp", l4[:, i, :], lam4[i:i + 1, :].partition_broadcast(128), writes=[b_c])
        P.dma("sp", dag[:], C.din("da_norm_g", [1, 128])[0:1, :].partition_broadcast(128), writes=[b_c])
        P.op("dve", lambda e: e.tensor_scalar(out=dag[:], in0=dag[:], scalar1=1.0 - LAMBDA_INIT, scalar2=None, op0=ALU.mult),
             reads=[b_c], writes=[b_c])
        P.op("dve", lambda e: e.tensor_tensor(out=lt[:, 0, :], in0=l4[:, 0, :], in1=l4[:, 1, :], op=ALU.mult), reads=[b_c], writes=[b_c])
        P.op("dve", lambda e: e.tensor_tensor(out=lt[:, 1, :], in0=l4[:, 2, :], in1=l4[:, 3, :], op=ALU.mult), reads=[b_c], writes=[b_c])
        P.op("dve", lambda e: e.tensor_reduce(out=lam[:, 0:2], in_=lt[:], axis=AX.X, op=ALU.add), reads=[b_c], writes=[b_c])
        P.op("act", lambda e: e.activation(out=lam[:, 0:2], in_=lam[:, 0:2], func=AF.Exp), reads=[b_c], writes=[b_c])
        P.op("dve", lambda e: e.tensor_tensor(out=lam[:, 2:3], in0=lam[:, 1:2], in1=lam[:, 0:1], op=ALU.subtract), reads=[b_c], writes=[b_c])
        P.op("dve", lambda e: e.tensor_scalar(out=lam[:, 2:3], in0=lam[:, 2:3], scalar1=-LAMBDA_INIT, scalar2=None, op0=ALU.add),
             reads=[b_c], writes=[b_c])
        NSC = 5
        PT = [sb(nc, st, f"at_PT{i}", [128, 512], BF16) for i in range(NSC)]
        PTb = [Buf() for _ in range(NSC)]
        tmpb_t = [sb(nc, st, f"at_tmp{i}", [128, 512], F32) for i in range(2)]
        tmpb = [Buf() for _ in range(2)]
        daoT = [sb(nc, st, f"at_daoT{i}", [128, TOWN], BF16) for i in range(2)]
        daoTb = [Buf() for _ in range(2)]
        o0 = [sb(nc, st, f"at_o0{i}", [128, 128], F32) for i in range(2)]
        o0b = [Buf() for _ in range(2)]
        ob = [sb(nc, st, f"at_ob{i}", [128, 128], BF16) for i in range(2)]
        obb = [Buf() for _ in range(2)]
        rs = sb(nc, st, "at_rs", [128, 8], F32)
        b_rs = Buf()
        r0 = sb(nc, st, "at_r0", [128, 4], F32)
        r1 = sb(nc, st, "at_r1", [128, 4], F32)
        o_all = sb(nc, st, "at_oall", [128, 4, 128], F32)
        t_all = sb(nc, st, "at_tall", [128, 4, 128], F32)
        ob_all = sb(nc, st, "at_oball", [128, 4, 128], BF16)
        b_oall, b_tall, b_oball = Buf(), Buf(), Buf()
        junk = sb(nc, st, "at_junk", [128, 128], BF16)
        b_junk = Buf()
        accA = [C.ps[0], C.ps[1]]
        accB = C.ps[2]
        accb = [C.psb[0], C.psb[1], C.psb[2]]
        sps = [C.ps[3][:], C.ps[4][:], C.ps[5][:], C.pst[0][:].bitcast(F32), C.pst[1][:].bitcast(F32)]
        spsb = [C.psb[3], C.psb[4], C.psb[5], C.pstb[0], C.pstb[1]]
        n_s = 0
        n_pt = 0
        n_tmp = 0
        n_o = 0
        for h in range(8):
            s = h % 2
            P.dma("sp", KT[s][:], KT_d[h, :, :], writes=[hb[s]])
            P.dma("sp", QT[s][:], QT_d[h, :, :], writes=[hb[s]])
            P.dma("sp", V[s][:], V_d[h, :, :, :], writes=[hb[s]])
            P.dma("sp", Wb[s][:], abias[h, :, :], writes=[hb[s]])
            for qg in range(4):
                kb_near0 = 16 + qg * 4 - 1
                nkb = 16 + 4 * (qg + 1)
                def emit_qk_exp(kb, comp, h=h, qg=qg, s=s, kb_near0=kb_near0):
                    nonlocal n_s, n_pt, n_tmp
                    pr = slice(comp * 64, (comp + 1) * 64)
                    si = n_s % NSC
                    n_s += 1
                    P.op("pe", lambda e: e.matmul(
                        sps[si], lhsT=KT[s][pr, kb * 128:(kb + 1) * 128], rhs=QT[s][pr, qg * 512:(qg + 1) * 512],
                        start=True, stop=True), reads=[hb[s]], writes=[spsb[si]])
                    pi = n_pt % NSC
                    n_pt += 1
                    if kb < kb_near0:
                        fcol = 2 * h + (0 if kb < 16 else 1)
                        P.op("act", lambda e: e.activation(
                            out=PT[pi][:], in_=sps[si], func=AF.Exp, bias=fb[:, fcol:fcol + 1]),
                            reads=[spsb[si], b_c], writes=[PTb[pi]])
                    else:
                        j = kb - kb_near0
                        u0 = 512 - 128 * j
                        ti = n_tmp % 2
                        n_tmp += 1
                        if kb < 16:
                            P.op("dve", lambda e: e.scalar_tensor_tensor(
                                out=tmpb_t[ti][:], in0=sps[si], scalar=fb[:, 16:17], in1=Wb[s][:, u0:u0 + 512],
                                op0=ALU.add, op1=ALU.add), reads=[spsb[si], hb[s], b_c], writes=[tmpb[ti]])
                        else:
                            P.op("dve", lambda e: e.tensor_tensor(
                                out=tmpb_t[ti][:], in0=sps[si], in1=Wb[s][:, u0:u0 + 512], op=ALU.add),
                                reads=[spsb[si], hb[s]], writes=[tmpb[ti]])
                        P.op("act", lambda e: e.activation(out=PT[pi][:], in_=tmpb_t[ti][:], func=AF.Exp),
                             reads=[tmpb[ti]], writes=[PTb[pi]])
                    return pi

                def emit_pv(kb, comp, pi, s=s, nkb=nkb):
                    def pv(e):
                        ins = None
                        for sub in range(int(os.environ.get("ATT_PVSUBS", "4"))):
                            if sub < 3:
                                out = accA[comp][:, sub * 129:(sub + 1) * 129]
                                first = (kb == 0 and sub == 0)
                            else:
                                out = accB[:, comp * 129:(comp + 1) * 129]
                                first = (kb == 0 and comp == 0)
                            ins = e.matmul(out, lhsT=PT[pi][:, sub * 128:(sub + 1) * 128], rhs=V[s][:, kb, :],
                                           start=first, stop=(kb == nkb - 1), skip_group_check=True)
                        return ins
                    P.op("pe", pv, reads=[PTb[pi], hb[s]], writes=[accb[comp], accb[2]])

                its = [(kb, comp) for kb in range(nkb) for comp in range(2)]
                LA = 4
                pend = []
                for (kb, comp) in its:
                    pi = emit_qk_exp(kb, comp)
                    pend.append((kb, comp, pi))
                    if len(pend) > LA:
                        emit_pv(*pend.pop(0))
                while pend:
                    emit_pv(*pend.pop(0))
                if C.debug is not None and C.debug[0] == "attn_acc" and (h, qg) == (0, 0):
                    P.barrier()
                    for bi in range(3):
                        P.op("dve", lambda e, bi=bi: e.tensor_copy(out=tmpb_t[0][:], in_=C.ps[bi][:]), reads=[accb[bi]], writes=[tmpb[0]])
                        P.dma("sp", C.dbg[bi], tmpb_t[0][:], reads=[tmpb[0]], is_out=True)
                    P.finish()
                    C.stopped = True
                    return
                pt_o, ptb_o = next_pst(C)
                A0 = accA[0][:, 0:387].rearrange("p (s c) -> p s c", c=129)
                A1 = accA[1][:, 0:387].rearrange("p (s c) -> p s c", c=129)
                B2 = accB[:, 0:258].rearrange("p (s c) -> p s c", c=129)
                Fd = lambda fn, r=(), w=(): P.op("dve", fn, reads=list(r), writes=list(w))
                Fd(lambda e: e.reciprocal(out=r0[:, 0:3], in_=A0[:, :, 128]), [accb[0]], [b_rs])
                Fd(lambda e: e.reciprocal(out=r0[:, 3:4], in_=B2[:, 0, 128:129]), [accb[2]], [b_rs])
                Fd(lambda e: e.reciprocal(out=r1[:, 0:3], in_=A1[:, :, 128]), [accb[1]], [b_rs])
                Fd(lambda e: e.reciprocal(out=r1[:, 3:4], in_=B2[:, 1, 128:129]), [accb[2]], [b_rs])
                Fd(lambda e: e.tensor_scalar(out=r1[:], in0=r1[:], scalar1=lam[:, 2:3], scalar2=None, op0=ALU.mult), [b_c], [b_rs])
                Fd(lambda e: e.tensor_tensor(out=o_all[:, 0:3, :], in0=A0[:, :, 0:128],
                                             in1=r0[:, 0:3].unsqueeze(2).to_broadcast([128, 3, 128]), op=ALU.mult), [accb[0], b_rs], [b_oall])
                Fd(lambda e: e.tensor_scalar(out=o_all[:, 3, :], in0=B2[:, 0, 0:128], scalar1=r0[:, 3:4], scalar2=None, op0=ALU.mult),
                   [accb[2], b_rs], [b_oall])
                Fd(lambda e: e.tensor_tensor(out=t_all[:, 0:3, :], in0=A1[:, :, 0:128],
                                             in1=r1[:, 0:3].unsqueeze(2).to_broadcast([128, 3, 128]), op=ALU.mult), [accb[1], b_rs], [b_tall])
                Fd(lambda e: e.tensor_scalar(out=t_all[:, 3, :], in0=B2[:, 1, 0:128], scalar1=r1[:, 3:4], scalar2=None, op0=ALU.mult),
                   [accb[2], b_rs], [b_tall])
                Fd(lambda e: e.tensor_tensor(out=o_all[:], in0=o_all[:], in1=t_all[:], op=ALU.add), [b_tall], [b_oall])
                Fd(lambda e: e.tensor_tensor(out=t_all[:], in0=o_all[:], in1=o_all[:], op=ALU.mult), [b_oall], [b_tall])
                Fd(lambda e: e.tensor_reduce(out=rs[:, 0:4], in_=t_all[:], axis=AX.X, op=ALU.add), [b_tall], [b_rs])
                P.op("act", lambda e: e.activation(out=rs[:, 4:8], in_=rs[:, 0:4], func=AF.Ln, scale=1.0 / 128, bias=C.cst[:, 0:1]),
                     reads=[C.b_cst], writes=[b_rs])
                P.op("act", lambda e: e.activation(out=rs[:, 4:8], in_=rs[:, 4:8], func=AF.Exp, scale=-0.5), reads=[], writes=[b_rs])
                Fd(lambda e: e.tensor_tensor(out=t_all[:], in0=o_all[:], in1=rs[:, 4:8].unsqueeze(2).to_broadcast([128, 4, 128]), op=ALU.mult),
                   [b_oall, b_rs], [b_tall])
                Fd(lambda e: e.tensor_tensor(out=ob_all[:], in0=t_all[:], in1=dag[:].unsqueeze(1).to_broadcast([128, 4, 128]), op=ALU.mult),
                   [b_tall, b_c], [b_oball])

                def tr4(e, pt_o=pt_o):
                    ins = None
                    for sub in range(4):
                        ins = e.transpose(out=pt_o[:, sub * 128:(sub + 1) * 128], in_=ob_all[:, sub, :], identity=C.ident[:])
                    return ins
                P.op("pe", tr4, reads=[b_oball, C.b_ident], writes=[ptb_o])
                P.barrier()
                P.op("act", lambda e, pt_o=pt_o, qg=qg, s=s: e.copy(out=daoT[s][:, qg * 512:(qg + 1) * 512], in_=pt_o[:, 0:512]),
                     reads=[ptb_o], writes=[daoTb[s]])
            P.dma("sp", daoT_d[:, h, :], daoT[s][:], reads=[daoTb[s]])
        P.barrier()


def load_rows_w(C, slot, slotb, w_ap, nk, col0, ncol):
    wv = w_ap.rearrange("(c p) n -> p c n", p=128)
    C.P.dma("pool", slot[:, 0:nk, 0:ncol], wv[:, :, col0:col0 + ncol], writes=[slotb])


def phase4_mix_out(C):
    nc, P = C.nc, C.P
    hgoT_d = C.dscr("hgoT", [128, 8, TOWN], BF16)
    daoT_d = C.dscr("daoT", [128, 8, TOWN], BF16)
    SG_d = C.dscr("SG", [2, 16, 128, TOWN], BF16)
    x1_d = C.dscr("x1", [TOWN, D], F32)
    xw = C.din("xw", [TWIN, D])
    wa = C.din("w_branch_a", [1024, D])
    wb_ = C.din("w_branch_b", [1024, D])
    wo = C.din("w_out", [D, D])
    with ExitStack() as st:
        hgoT = sb(nc, st, "p4_hgoT", [128, 8, TOWN], BF16)
        daoT = sb(nc, st, "p4_daoT", [128, 8, TOWN], BF16)
        mT = sb(nc, st, "p4_mT", [128, NCH, TOWN], BF16)
        b_in = Buf()
        mTb = [Buf() for _ in range(4)]
        P.dma("sp", hgoT[:], hgoT_d[:, :, :], writes=[b_in])
        P.dma("sp", daoT[:], daoT_d[:, :, :], writes=[b_in])
        with ExitStack() as s2:
            was = [sb(nc, s2, f"p4_wa{i}", [128, 8, 512], BF16) for i in range(2)]
            wbs = [sb(nc, s2, f"p4_wb{i}", [128, 8, 512], BF16) for i in range(2)]
            wab = [Buf() for _ in range(2)]
            wbb = [Buf() for _ in range(2)]
            sga = [sb(nc, s2, f"p4_sga{i}", [128, TOWN], BF16) for i in range(2)]
            sgb_ = [sb(nc, s2, f"p4_sgb{i}", [128, TOWN], BF16) for i in range(2)]
            sgab = [Buf() for _ in range(2)]
            sgbb = [Buf() for _ in range(2)]
            t1 = [sb(nc, s2, f"p4_t1{i}", [128, 512], F32) for i in range(2)]
            t2 = [sb(nc, s2, f"p4_t2{i}", [128, 512], F32) for i in range(2)]
            t1b = [Buf() for _ in range(2)]
            t2b = [Buf() for _ in range(2)]
            n = 0
            for cg in range(4):
                ws = cg % 2
                load_rows_w(C, was[ws], wab[ws], wa, 8, cg * 512, 512)
                load_rows_w(C, wbs[ws], wbb[ws], wb_, 8, cg * 512, 512)
                for cc in range(4):
                    ch = cg * 4 + cc
                    gs = ch % 2
                    P.dma("sp", sga[gs][:], SG_d[0, ch, :, :], writes=[sgab[gs]])
                    P.dma("sp", sgb_[gs][:], SG_d[1, ch, :, :], writes=[sgbb[gs]])
                    for tg in range(4):
                        sl = slice(tg * 512, (tg + 1) * 512)
                        ti = n % 2
                        n += 1
                        for (wsl_, wslb_, src, tt, ttb, sg_, sgb2) in ((was[ws], wab[ws], hgoT, t1, t1b, sga[gs], sgab[gs]),
                                                                      (wbs[ws], wbb[ws], daoT, t2, t2b, sgb_[gs], sgbb[gs])):
                            ps, psb = next_ps(C)

                            def mm(e, ps=ps, wsl_=wsl_, src=src, cc=cc, sl=sl):
                                ins = None
                                for k in range(8):
                                    ins = e.matmul(ps[:], lhsT=wsl_[:, k, cc * 128:(cc + 1) * 128], rhs=src[:, k, sl],
                                                   start=(k == 0), stop=(k == 7))
                                return ins
                            P.op("pe", mm, reads=[wslb_, b_in], writes=[psb])
                            P.op("dve", lambda e, ps=ps, tt=tt, ti=ti, sg_=sg_, sl=sl: e.tensor_tensor(
                                out=tt[ti][:], in0=ps[:], in1=sg_[:, sl], op=ALU.mult), reads=[psb, sgb2], writes=[ttb[ti]])
                        P.op("dve", lambda e, ti=ti, ch=ch, sl=sl: e.tensor_tensor(
                            out=mT[:, ch, sl], in0=t1[ti][:], in1=t2[ti][:], op=ALU.add),
                            reads=[t1b[ti], t2b[ti]], writes=[mTb[tg]])
            P.barrier()
        if C.debug is not None and C.debug[0] == "mT":
            P.barrier()
            P.dma("sp", C.dbg, mT[:], is_out=True)
            P.finish()
            C.stopped = True
            return
        with ExitStack() as s2:
            wos = [sb(nc, s2, f"p4_wo{i}", [128, NCH, 512], BF16) for i in range(2)]
            wob = [Buf() for _ in range(2)]
            g1 = sb(nc, s2, "p4_g1", [128, D], F32)
            b_g1 = Buf()
            P.dma("sp", g1[:], C.mod_d[0:1, 2 * D:3 * D].partition_broadcast(128), writes=[b_g1])
            xt = [sb(nc, s2, f"p4_xt{i}", [128, 512], F32) for i in range(3)]
            xtb = [Buf() for _ in range(3)]
            yt = [sb(nc, s2, f"p4_yt{i}", [128, 512], F32) for i in range(2)]
            ytb = [Buf() for _ in range(2)]
            n = 0
            for cg in range(4):
                ws = cg % 2
                load_rows_w(C, wos[ws], wob[ws], wo, NCH, cg * 512, 512)
                csl = slice(cg * 512, (cg + 1) * 512)
                for tt in range(16):
                    xi = n % 3
                    yi = n % 2
                    n += 1
                    P.dma("sp", xt[xi][:], xw[TOWN + tt * 128:TOWN + (tt + 1) * 128, csl], writes=[xtb[xi]])
                    ps, psb = next_ps(C)

                    def mm(e, ps=ps, ws=ws, tt=tt):
                        ins = None
                        for k in range(NCH):
                            ins = e.matmul(ps[:], lhsT=mT[:, k, tt * 128:(tt + 1) * 128], rhs=wos[ws][:, k, :],
                                           start=(k == 0), stop=(k == NCH - 1))
                        return ins
                    P.op("pe", mm, reads=[wob[ws], mTb[tt // 4]], writes=[psb])
                    P.op("dve", lambda e, ps=ps, yi=yi, csl=csl: e.tensor_tensor(out=yt[yi][:], in0=ps[:], in1=g1[:, csl], op=ALU.mult),
                         reads=[psb, b_g1], writes=[ytb[yi]])
                    P.op("dve", lambda e, yi=yi, xi=xi: e.tensor_tensor(out=xt[xi][:], in0=yt[yi][:], in1=xt[xi][:], op=ALU.add),
                         reads=[ytb[yi]], writes=[xtb[xi]])
                    P.dma("sp", x1_d[tt * 128:(tt + 1) * 128, csl], xt[xi][:], reads=[xtb[xi]])
            P.barrier()
        P.barrier()


CAP = 1024
NSLOT = 64 * CAP
BIG = 1.0e9


def phase5_route(C, st_keep):
    nc, P = C.nc, C.P
    x1_d = C.dscr("x1", [TOWN, D], F32)
    Xs_d = C.dscr("Xs", [NSLOT, D], BF16)
    Y_d = C.dscr("Y", [NSLOT, D], BF16)
    Ysh_d = C.dscr("Ysh", [TOWN, D], BF16)
    norm2_g = C.din("norm2_g", [1, D])
    C.bc_reg = nc.gpsimd.to_reg(NSLOT - 1)
    C.flag_i = sb(nc, st_keep, "flag_i", [1, 512], I32)
    C.idx_all = sb(nc, st_keep, "idx_all", [128, 128], I32)
    C.w_all = sb(nc, st_keep, "w_all", [128, 128], F32)
    C.b_idx = [Buf() for _ in range(16)]
    with ExitStack() as st:
        G = sb(nc, st, "n2_G", [128, D], F32)
        S = sb(nc, st, "n2_S", [128, D], F32)
        t0 = sb(nc, st, "n2_t0", [128, D], F32)
        bG, bS, bt0 = Buf(), Buf(), Buf()
        P.dma("sp", G[:], C.mod_d[0:1, 4 * D:5 * D].partition_broadcast(128), writes=[bG])
        P.dma("sp", t0[:], norm2_g[0:1, :].partition_broadcast(128), writes=[bt0])
        P.dma("sp", S[:], C.mod_d[0:1, 3 * D:4 * D].partition_broadcast(128), writes=[bS])
        P.op("dve", lambda e: e.scalar_tensor_tensor(out=G[:], in0=G[:], scalar=1.0, in1=t0[:], op0=ALU.add, op1=ALU.mult),
             reads=[bt0], writes=[bG])
        h2f, b_h2f = t0, bt0
        hTs = sb(nc, st, "p5_hTs", [128, NCH, 256], BF16)
        _hslots = [Buf(), Buf()]
        hTsb = [_hslots[i % 2] for i in range(16)]
        h2T32 = sb(nc, st, "p5_h2T32", [128, NCH, 128], F32)
        b_h2T32 = Buf()
        wr = sb(nc, st, "p5_wr", [128, NCH, 64], F32)
        rb_bc = sb(nc, st, "p5_rb", [128, 64], F32)
        ecap = sb(nc, st, "p5_ecap", [128, 64], F32)
        ustr_f = sb(nc, st, "p5_ustr_f", [128, 128], F32)
        ustr = sb(nc, st, "p5_ustr", [128, 128], BF16)
        ones_b = sb(nc, st, "p5_ones", [128, 128], BF16)
        base = sb(nc, st, "p5_base", [128, 64], F32)
        b_c, b_base = Buf(), Buf()
        P.dma("sp", wr[:], C.din("router_w", [D, 64]).rearrange("(c p) n -> p c n", p=128), writes=[b_c])
        P.dma("sp", rb_bc[:], C.din("router_bias", [1, 64])[0:1, :].partition_broadcast(128), writes=[b_c])
        P.dma("sp", ecap[:], C.din("ecap", [1, 64])[0:1, :].partition_broadcast(128), writes=[b_c])
        P.dma("sp", ustr_f[:], C.din("ustrict", [128, 128])[:, :], writes=[b_c])
        P.op("dve", lambda e: e.tensor_copy(out=ustr[:], in_=ustr_f[:]), reads=[b_c], writes=[b_c])
        P.op("dve", lambda e: e.memset(ones_b[:], 1.0), writes=[b_c])
        P.op("dve", lambda e: e.memset(base[:], 0.0), writes=[b_base])
        wsg = sb(nc, st, "p5_wsg", [128, NCH, 512], BF16)
        wsu = sb(nc, st, "p5_wsu", [128, NCH, 512], BF16)
        wsd = sb(nc, st, "p5_wsd", [128, 4, D], BF16)
        b_ws = Buf()
        load_rows_w(C, wsg, b_ws, C.din("w_sh_gate", [D, 512]), NCH, 0, 512)
        load_rows_w(C, wsu, b_ws, C.din("w_sh_up", [D, 512]), NCH, 0, 512)
        load_rows_w(C, wsd, b_ws, C.din("w_sh_down", [512, D]), 4, 0, D)
        def t(name, shape, dt=F32):
            return sb(nc, st, "p5_" + name, shape, dt)
        sc = t("sc", [128, 64]); sel = t("sel", [128, 64]); eq = t("eq", [128, 64]); sel2 = t("sel2", [128, 64])
        m1 = t("m1", [128, 8]); m2 = t("m2", [128, 8]); gs = t("gs", [128, 8]); t8 = t("t8", [128, 8])
        gm = t("gm", [128, 8]); pen = t("pen", [128, 8]); selm = t("selm", [128, 64]); em = t("em", [128, 64])
        emb = t("emb", [128, 64], BF16); gw = t("gw", [128, 64]); den = t("den", [128, 2]); Gt = t("Gt", [128, 64])
        pos = t("pos", [128, 64]); val = t("val", [128, 64]); v01 = t("v01", [128, 64]); top8 = t("top8", [128, 8])
        neg = t("neg", [128, 8]); idxf = t("idxf", [128, 8]); oh = t("oh", [128, 64]); ohj = t("ohj", [128, 64])
        b_r = Buf()
        sgs = t("sgs", [128, 128]); h1s = t("h1s", [128, 4, 128], BF16); ysh = [t(f"ysh{i}", [128, D], BF16) for i in range(2)]
        b_sgs, b_h1s = Buf(), Buf()
        yshb = [Buf() for _ in range(2)]

        def h32_cb(i, tmp, btmp, S_, bS_, hb_s, hbb_s):
            P.op("dve", lambda e: e.tensor_tensor(out=h2f[:], in0=tmp[:], in1=S_[:], op=ALU.add),
                 reads=[btmp, bS_], writes=[b_h2f])
            P.op("act", lambda e: e.copy(out=hb_s[:], in_=h2f[:]), reads=[b_h2f], writes=[hbb_s])
            for q4 in range(4):
                ps, psb = next_ps(C)

                def tr(e, ps=ps, q4=q4):
                    ins = None
                    for c in range(4):
                        cc = q4 * 4 + c
                        ins = e.transpose(out=ps[:, c * 128:(c + 1) * 128], in_=h2f[:, cc * 128:(cc + 1) * 128],
                                          identity=C.ident_f[:])
                    return ins
                P.op("pe", tr, reads=[b_h2f, C.b_ident], writes=[psb])
                P.op("act", lambda e, ps=ps, q4=q4: e.copy(out=h2T32[:, q4 * 4:(q4 + 1) * 4, :],
                                                           in_=ps[:].rearrange("p (c t) -> p c t", t=128)),
                     reads=[psb], writes=[b_h2T32])
            ps, psb = next_ps(C)

            def mmr(e, ps=ps):
                ins = None
                for c in range(NCH):
                    ins = e.matmul(ps[:, 0:64], lhsT=h2T32[:, c, :], rhs=wr[:, c, :], start=(c == 0), stop=(c == NCH - 1))
                return ins
            P.op("pe", mmr, reads=[b_h2T32, b_c], writes=[psb])
            D_ = lambda fn, extra=(): P.op("dve", fn, reads=[b_c] + list(extra), writes=[b_r])
            P.op("act", lambda e, ps=ps: e.activation(out=sc[:], in_=ps[:, 0:64], func=AF.Sigmoid), reads=[psb], writes=[b_r])
            D_(lambda e: e.tensor_tensor(out=sel[:], in0=sc[:], in1=rb_bc[:], op=ALU.add))
            g3 = lambda ap: ap.rearrange("p (g k) -> p g k", k=8)
            D_(lambda e: e.tensor_reduce(out=m1[:], in_=g3(sel[:]), axis=AX.X, op=ALU.max))
            D_(lambda e: e.tensor_tensor(out=g3(eq[:]), in0=g3(sel[:]), in1=m1[:].unsqueeze(2).to_broadcast([128, 8, 8]), op=ALU.is_equal))
            D_(lambda e: e.scalar_tensor_tensor(out=sel2[:], in0=eq[:], scalar=-BIG, in1=sel[:], op0=ALU.mult, op1=ALU.add))
            D_(lambda e: e.tensor_reduce(out=m2[:], in_=g3(sel2[:]), axis=AX.X, op=ALU.max))
            D_(lambda e: e.tensor_tensor(out=gs[:], in0=m1[:], in1=m2[:], op=ALU.add))
            D_(lambda e: e.max(out=t8[:], in_=gs[:]))
            D_(lambda e: e.tensor_scalar(out=gm[:], in0=gs[:], scalar1=t8[:, 3:4], scalar2=None, op0=ALU.is_ge))
            D_(lambda e: e.tensor_scalar(out=pen[:], in0=gm[:], scalar1=BIG, scalar2=-BIG, op0=ALU.mult, op1=ALU.add))
            D_(lambda e: e.tensor_tensor(out=g3(selm[:]), in0=g3(sel[:]), in1=gm[:].unsqueeze(2).to_broadcast([128, 8, 8]), op=ALU.mult))
            D_(lambda e: e.tensor_tensor(out=g3(selm[:]), in0=g3(selm[:]), in1=pen[:].unsqueeze(2).to_broadcast([128, 8, 8]), op=ALU.add))
            D_(lambda e: e.max(out=t8[:], in_=selm[:]))
            D_(lambda e: e.tensor_scalar(out=em[:], in0=selm[:], scalar1=t8[:, 7:8], scalar2=None, op0=ALU.is_ge))
            D_(lambda e: e.tensor_copy(out=emb[:], in_=em[:]))
            D_(lambda e: e.tensor_tensor(out=gw[:], in0=sc[:], in1=em[:], op=ALU.mult))
            D_(lambda e: e.tensor_reduce(out=den[:, 0:1], in_=gw[:], axis=AX.X, op=ALU.add))
            D_(lambda e: e.reciprocal(out=den[:, 1:2], in_=den[:, 0:1]))
            D_(lambda e: e.tensor_scalar(out=Gt[:], in0=gw[:], scalar1=den[:, 1:2], scalar2=2.5, op0=ALU.mult, op1=ALU.mult))
            psp, pspb = next_ps(C)
            P.op("pe", lambda e, psp=psp: e.matmul(psp[:, 0:64], lhsT=ustr[:], rhs=emb[:], start=True, stop=True),
                 reads=[b_r, b_c], writes=[pspb])
            P.op("dve", lambda e, psp=psp: e.tensor_tensor(out=pos[:], in0=psp[:, 0:64], in1=base[:], op=ALU.add),
                 reads=[pspb, b_base], writes=[b_r])
            pst_, pstb_ = next_ps(C)
            P.op("pe", lambda e, pst_=pst_: e.matmul(pst_[:, 0:64], lhsT=ones_b[:], rhs=emb[:], start=True, stop=True),
                 reads=[b_r, b_c], writes=[pstb_])
            P.op("dve", lambda e, pst_=pst_: e.tensor_tensor(out=base[:], in0=pst_[:, 0:64], in1=base[:], op=ALU.add),
                 reads=[pstb_, b_r], writes=[b_base])
            D_(lambda e: e.tensor_scalar(out=v01[:], in0=pos[:], scalar1=float(CAP), scalar2=None, op0=ALU.is_lt))
            D_(lambda e: e.tensor_tensor(out=v01[:], in0=v01[:], in1=em[:], op=ALU.mult))
            D_(lambda e: e.scalar_tensor_tensor(out=val[:], in0=pos[:], scalar=1.0, in1=ecap[:], op0=ALU.add, op1=ALU.add))
            D_(lambda e: e.tensor_tensor(out=val[:], in0=val[:], in1=v01[:], op=ALU.mult))
            D_(lambda e: e.tensor_scalar(out=val[:], in0=val[:], scalar1=-1.0, scalar2=None, op0=ALU.add))
            D_(lambda e: e.max(out=top8[:], in_=val[:]))
            D_(lambda e: e.tensor_scalar(out=neg[:], in0=top8[:], scalar1=0.0, scalar2=None, op0=ALU.is_lt))
            D_(lambda e: e.scalar_tensor_tensor(out=idxf[:], in0=neg[:], scalar=4.0e6, in1=top8[:], op0=ALU.mult, op1=ALU.add))
            P.op("dve", lambda e: e.tensor_copy(out=C.idx_all[:, i * 8:(i + 1) * 8], in_=idxf[:]), reads=[b_r], writes=[C.b_idx[i]])
            for k in range(8):
                D_(lambda e, k=k: e.tensor_scalar(out=oh[:], in0=val[:], scalar1=top8[:, k:k + 1], scalar2=None, op0=ALU.is_equal))
                D_(lambda e: e.tensor_tensor(out=ohj[:], in0=oh[:], in1=Gt[:], op=ALU.mult))
                P.op("dve", lambda e, k=k: e.tensor_reduce(out=C.w_all[:, i * 8 + k:i * 8 + k + 1], in_=ohj[:], axis=AX.X, op=ALU.add),
                     reads=[b_r], writes=[C.b_idx[i]])
            D_(lambda e: e.tensor_scalar(out=neg[:], in0=neg[:], scalar1=-1.0, scalar2=1.0, op0=ALU.mult, op1=ALU.add))
            P.op("dve", lambda e: e.tensor_tensor(out=C.w_all[:, i * 8:(i + 1) * 8], in0=C.w_all[:, i * 8:(i + 1) * 8], in1=neg[:], op=ALU.mult),
                 reads=[b_r], writes=[C.b_idx[i]])
            if os.environ.get("IND_PROBE") and i == 0:
                with ExitStack() as sp_:
                    it_ = sb(nc, sp_, "prb_it", [128, 128], I32)
                    xb_ = sb(nc, sp_, "prb_xb", [128, D], BF16)
                    sem_ = P.q["pool"].ring[0].sem
                    for nm, idx_ap, in_ap in (("fresh/fresh", it_[:, 3:4], xb_[:, :]), ("idx_all/fresh", C.idx_all[:, 3:4], xb_[:, :]),
                                              ("fresh/hb", it_[:, 3:4], hb_s[:, :]), ("idx_all/hb", C.idx_all[:, 0:1], hb_s[:, :])):
                        try:
                            nc.gpsimd.indirect_dma_start(out=Xs_d[:, :], out_offset=bass.IndirectOffsetOnAxis(ap=idx_ap, axis=0),
                                                         in_=in_ap, in_offset=None, bounds_check=C.bc_reg, oob_is_err=False).then_inc(sem_, 16)
                            print("IND_PROBE ok", nm)
                        except Exception as ex:
                            print("IND_PROBE fail", nm, ex)
            for k in range(8):
                if os.environ.get("IND_PROBE"):
                    print("IND_PROBE emitting scatter", i, k, flush=True)
                P.dma_custom("pool", lambda e, k=k: e.indirect_dma_start(
                    out=Xs_d[:, :], out_offset=bass.IndirectOffsetOnAxis(ap=C.idx_all[:, i * 8 + k:i * 8 + k + 1], axis=0),
                    in_=hb_s[:, :], in_offset=None, bounds_check=C.bc_reg, oob_is_err=False),
                    reads=[C.b_idx[i], hbb_s])

        def post_cb(i, hb_s, hbb_s, c0):
            for fc in range(4):
                psg, psgb = next_ps(C)
                psu, psub = next_ps(C)
                for (ps_, psb_, w_) in ((psg, psgb, wsg), (psu, psub, wsu)):
                    def mm(e, ps_=ps_, w_=w_, fc=fc):
                        ins = None
                        for c in range(NCH):
                            ins = e.matmul(ps_[:, 0:128], lhsT=w_[:, c, fc * 128:(fc + 1) * 128], rhs=hTs[:, c, c0:c0 + 128],
                                           start=(c == 0), stop=(c == NCH - 1))
                        return ins
                    P.op("pe", mm, reads=[b_ws, hTsb[i]], writes=[psb_])
                P.op("act", lambda e, psg=psg: e.activation(out=sgs[:], in_=psg[:, 0:128], func=AF.Silu), reads=[psgb], writes=[b_sgs])
                P.op("dve", lambda e, psu=psu, fc=fc: e.tensor_tensor(out=h1s[:, fc, :], in0=sgs[:], in1=psu[:, 0:128], op=ALU.mult),
                     reads=[b_sgs, psub], writes=[b_h1s])
            yi = i % 2
            for cg in range(4):
                ps, psb = next_ps(C)

                def mm(e, ps=ps, cg=cg):
                    ins = None
                    for fc in range(4):
                        ins = e.matmul(ps[:], lhsT=h1s[:, fc, :], rhs=wsd[:, fc, cg * 512:(cg + 1) * 512],
                                       start=(fc == 0), stop=(fc == 3))
                    return ins
                P.op("pe", mm, reads=[b_h1s, b_ws], writes=[psb])
                P.op("act", lambda e, ps=ps, cg=cg, yi=yi: e.copy(out=ysh[yi][:, cg * 512:(cg + 1) * 512], in_=ps[:]),
                     reads=[psb], writes=[yshb[yi]])
            P.dma("sp", Ysh_d[i * 128:(i + 1) * 128, :], ysh[yi][:], reads=[yshb[yi]])

        norm_tiles(C, st, lambda i: x1_d[i * 128:(i + 1) * 128, :], G, bG, S, bS, hTs, hTsb, "n2",
                   h32_cb=h32_cb, col_fn=lambda i: (i % 2) * 128, post_cb=post_cb)
        thr = sb(nc, st, "p5_thr", [1, 512], F32)
        flf = sb(nc, st, "p5_flf", [1, 512], F32)
        b_f = Buf()
        P.dma("sp", thr[:], C.din("thr8", [1, 512])[:, :], writes=[b_f])
        P.op("dve", lambda e: e.tensor_tensor(out=flf[:].rearrange("p (e b) -> p e b", b=8),
                                              in0=base[0:1, :].unsqueeze(2).to_broadcast([1, 64, 8]),
                                              in1=thr[:].rearrange("p (e b) -> p e b", b=8), op=ALU.is_gt),
             reads=[b_base, b_f], writes=[b_f])
        P.op("dve", lambda e: e.tensor_copy(out=C.flag_i[:], in_=flf[:]), reads=[b_f], writes=[b_f])
        P.barrier()


def phase6_experts(C):
    nc, P = C.nc, C.P
    Xs_d = C.dscr("Xs", [NSLOT, D], BF16)
    Y_d = C.dscr("Y", [NSLOT, D], BF16)
    Ysh_d = C.dscr("Ysh", [TOWN, D], BF16)
    weg = C.din("w_exp_gate", [64, D, 512])
    weu = C.din("w_exp_up", [64, D, 512])
    wed = C.din("w_exp_down", [64, 512, D])
    with ExitStack() as st:
        wg = [sb(nc, st, f"p6_wg{i}", [128, NCH, 512], BF16) for i in range(2)]
        wu = [sb(nc, st, f"p6_wu{i}", [128, NCH, 512], BF16) for i in range(2)]
        wd = [sb(nc, st, f"p6_wd{i}", [128, 4, D], BF16) for i in range(2)]
        wb = [Buf() for _ in range(2)]
        xtok = [sb(nc, st, f"p6_xtok{i}", [128, D], BF16) for i in range(3)]
        xtokb = [Buf() for _ in range(3)]
        XT = [sb(nc, st, f"p6_XT{i}", [128, NCH, 512], BF16) for i in range(2)]
        XTb = [Buf() for _ in range(2)]
        sg = [sb(nc, st, f"p6_sg{i}", [128, 512], F32) for i in range(2)]
        sgb = [Buf() for _ in range(2)]
        h1 = [sb(nc, st, f"p6_h1{i}", [128, 4, 512], BF16) for i in range(2)]
        h1b = [Buf() for _ in range(2)]
        yev = [sb(nc, st, f"p6_yev{i}", [128, D], BF16) for i in range(2)]
        yevb = [Buf() for _ in range(2)]
        n_x = 0
        n_g = 0
        n_y = 0
        n_sg = 0
        NG = CAP // 512
        for e in range(int(os.environ.get("P6_NEXP", "64"))):
            ws = e % 2
            if not (os.environ.get("P6_NOW") and e >= 2):
                load_rows_w(C, wg[ws], wb[ws], weg[e], NCH, 0, 512)
                load_rows_w(C, wu[ws], wb[ws], weu[e], NCH, 0, 512)
                load_rows_w(C, wd[ws], wb[ws], wed[e], 4, 0, D)
            for g2 in range(NG):
                gi = n_g % 2
                n_g += 1
                slot0 = e * CAP + g2 * 512
                for blk in range(4):
                    xi = n_x % 3
                    n_x += 1
                    r0 = slot0 + blk * 128
                    P.dma_cond(C.flag_i[0:1, e * 8 + g2 * 4 + blk:e * 8 + g2 * 4 + blk + 1], xtok[xi][:], Xs_d[r0:r0 + 128, :],
                               writes=[xtokb[xi]])
                    for hlf in range(2):
                        pt, ptb = next_pst(C)

                        def tr(e_, xi=xi, hlf=hlf, pt=pt):
                            ins = None
                            for c in range(8):
                                cc = hlf * 8 + c
                                ins = e_.transpose(out=pt[:, c * 128:(c + 1) * 128], in_=xtok[xi][:, cc * 128:(cc + 1) * 128],
                                                   identity=C.ident[:])
                            return ins
                        P.op_cond(C.flag_i[0:1, e * 8 + g2 * 4 + blk:e * 8 + g2 * 4 + blk + 1], tr,
                                  reads=[xtokb[xi], C.b_ident], writes=[ptb])
                        eng = "act" if hlf == 0 else "dve"
                        if eng == "act":
                            P.op_cond(C.flag_i[0:1, e * 8 + g2 * 4 + blk:e * 8 + g2 * 4 + blk + 1], lambda e_, gi=gi, hlf=hlf, blk=blk, pt=pt: e_.copy(
                                out=XT[gi][:, hlf * 8:(hlf + 1) * 8, blk * 128:(blk + 1) * 128],
                                in_=pt[:].rearrange("p (c t) -> p c t", t=128)), reads=[ptb], writes=[XTb[gi]], qname="act")
                        else:
                            P.op_cond(C.flag_i[0:1, e * 8 + g2 * 4 + blk:e * 8 + g2 * 4 + blk + 1], lambda e_, gi=gi, hlf=hlf, blk=blk, pt=pt: e_.tensor_copy(
                                out=XT[gi][:, hlf * 8:(hlf + 1) * 8, blk * 128:(blk + 1) * 128],
                                in_=pt[:].rearrange("p (c t) -> p c t", t=128)), reads=[ptb], writes=[XTb[gi]], qname="dve")
                for fc in range(4):
                    psg, psgb = next_ps(C)
                    psu, psub = next_ps(C)
                    for (ps_, psb_, w_) in ((psg, psgb, wg[ws]), (psu, psub, wu[ws])):
                        def mm(e_, ps_=ps_, w_=w_, fc=fc, gi=gi):
                            ins = None
                            for c in range(NCH):
                                ins = e_.matmul(ps_[:], lhsT=w_[:, c, fc * 128:(fc + 1) * 128], rhs=XT[gi][:, c, :],
                                                start=(c == 0), stop=(c == NCH - 1))
                            return ins
                        P.op_cond(C.flag_i[0:1, e * 8 + g2 * 4:e * 8 + g2 * 4 + 1], mm, reads=[wb[ws], XTb[gi]], writes=[psb_])
                    si = n_sg % 2
                    n_sg += 1
                    P.op_cond(C.flag_i[0:1, e * 8 + g2 * 4:e * 8 + g2 * 4 + 1], lambda e_, psg=psg, si=si: e_.activation(out=sg[si][:], in_=psg[:], func=AF.Silu),
                              reads=[psgb], writes=[sgb[si]], qname="act")
                    P.op_cond(C.flag_i[0:1, e * 8 + g2 * 4:e * 8 + g2 * 4 + 1], lambda e_, psu=psu, si=si, gi=gi, fc=fc: e_.tensor_tensor(
                        out=h1[gi][:, fc, :], in0=sg[si][:], in1=psu[:], op=ALU.mult),
                        reads=[sgb[si], psub], writes=[h1b[gi]], qname="dve")
                for blk in range(4):
                    yi = n_y % 2
                    n_y += 1
                    for cg in range(4):
                        ps, psb = next_ps(C)

                        def mm(e_, ps=ps, gi=gi, blk=blk, cg=cg, ws=ws):
                            ins = None
                            for fc in range(4):
                                ins = e_.matmul(ps[:], lhsT=h1[gi][:, fc, blk * 128:(blk + 1) * 128],
                                                rhs=wd[ws][:, fc, cg * 512:(cg + 1) * 512], start=(fc == 0), stop=(fc == 3))
                            return ins
                        P.op_cond(C.flag_i[0:1, e * 8 + g2 * 4 + blk:e * 8 + g2 * 4 + blk + 1], mm,
                                  reads=[h1b[gi], wb[ws]], writes=[psb])
                        if cg % 2 == 0:
                            P.op_cond(C.flag_i[0:1, e * 8 + g2 * 4 + blk:e * 8 + g2 * 4 + blk + 1], lambda e_, ps=ps, yi=yi, cg=cg: e_.copy(out=yev[yi][:, cg * 512:(cg + 1) * 512], in_=ps[:]),
                                      reads=[psb], writes=[yevb[yi]], qname="act")
                        else:
                            P.op_cond(C.flag_i[0:1, e * 8 + g2 * 4 + blk:e * 8 + g2 * 4 + blk + 1], lambda e_, ps=ps, yi=yi, cg=cg: e_.tensor_copy(out=yev[yi][:, cg * 512:(cg + 1) * 512], in_=ps[:]),
                                      reads=[psb], writes=[yevb[yi]], qname="dve")
                    r0 = slot0 + blk * 128
                    P.dma_cond(C.flag_i[0:1, e * 8 + g2 * 4 + blk:e * 8 + g2 * 4 + blk + 1], Y_d[r0:r0 + 128, :], yev[yi][:],
                               reads=[yevb[yi]])
        P.barrier()


def phase7_combine(C):
    nc, P = C.nc, C.P
    x1_d = C.dscr("x1", [TOWN, D], F32)
    Y_d = C.dscr("Y", [NSLOT, D], BF16)
    Ysh_d = C.dscr("Ysh", [TOWN, D], BF16)
    with ExitStack() as st:
        g2 = sb(nc, st, "p7_g2", [128, D], F32)
        b_g2 = Buf()
        P.dma("sp", g2[:], C.mod_d[0:1, 5 * D:6 * D].partition_broadcast(128), writes=[b_g2])
        yg = [sb(nc, st, f"p7_yg{i}", [128, D], BF16) for i in range(4)]
        ygb = [Buf() for _ in range(4)]
        for i in range(4):
            P.op("dve", lambda e, i=i: e.memset(yg[i][:], 0.0), writes=[ygb[i]])
        ysh = [sb(nc, st, f"p7_ysh{i}", [128, D], BF16) for i in range(2)]
        yshb = [Buf() for _ in range(2)]
        x1t = [sb(nc, st, f"p7_x1{i}", [128, D], F32) for i in range(2)]
        x1b = [Buf() for _ in range(2)]
        acc = [sb(nc, st, f"p7_acc{i}", [128, D], F32) for i in range(2)]
        accb = [Buf() for _ in range(2)]
        n_g = 0
        for i in range(16):
            s = i % 2
            P.dma("sp", ysh[s][:], Ysh_d[i * 128:(i + 1) * 128, :], writes=[yshb[s]])
            P.dma("sp", x1t[s][:], x1_d[i * 128:(i + 1) * 128, :], writes=[x1b[s]])
            for k in range(8):
                gi = n_g % 4
                n_g += 1
                P.dma_custom("pool", lambda e, gi=gi, k=k, i=i: e.indirect_dma_start(
                    out=yg[gi][:, :], out_offset=None, in_=Y_d[:, :],
                    in_offset=bass.IndirectOffsetOnAxis(ap=C.idx_all[:, i * 8 + k:i * 8 + k + 1], axis=0),
                    bounds_check=C.bc_reg, oob_is_err=False), reads=[C.b_idx[i]], writes=[ygb[gi]])
                src = ysh[s] if k == 0 else acc[s]
                srcb = yshb[s] if k == 0 else accb[s]
                P.op("dve", lambda e, gi=gi, k=k, i=i, src=src, s=s: e.scalar_tensor_tensor(
                    out=acc[s][:], in0=yg[gi][:], scalar=C.w_all[:, i * 8 + k:i * 8 + k + 1], in1=src[:], op0=ALU.mult, op1=ALU.add),
                    reads=[ygb[gi], C.b_idx[i], srcb], writes=[accb[s]])
            P.op("dve", lambda e, s=s: e.tensor_tensor(out=acc[s][:], in0=acc[s][:], in1=g2[:], op=ALU.mult),
                 reads=[b_g2], writes=[accb[s]])
            P.op("dve", lambda e, s=s: e.tensor_tensor(out=acc[s][:], in0=acc[s][:], in1=x1t[s][:], op=ALU.add),
                 reads=[x1b[s]], writes=[accb[s]])
            P.dma("sp", C.out[i * 128:(i + 1) * 128, :], acc[s][:], reads=[accb[s]], is_out=True)
        P.barrier()
```
